# Optimizing a Trainium2 kernel written in Bass

```python
import math
import jax, jax.numpy as jnp
from jax import lax
import numpy as np

D_MODEL = 1024
BATCH = 4
SEQ = 8192
DEPTH = 1

CHUNK = 64
Q_BLOCK = 128
POOL_WINDOWS = (2, 4, 8, 16)
POOL_GROUPS = len(POOL_WINDOWS)
POOL_WIDTH = D_MODEL // 2
POOL_GROUP_DIM = POOL_WIDTH // POOL_GROUPS
POOL_OUT_GROUP_DIM = D_MODEL // POOL_GROUPS
N_HEADS = 8
QK_NOPE_DIM = D_MODEL // 16
QK_ROPE_DIM = D_MODEL // 32
V_HEAD_DIM = D_MODEL // 16
Q_LORA_RANK = 3 * D_MODEL // 8
KV_LORA_RANK = D_MODEL // 4
ROPE_THETA = 10000.0
N_BRANCHES = 2
IN_SPLITS = (POOL_WIDTH,
             POOL_WIDTH + Q_LORA_RANK,
             POOL_WIDTH + Q_LORA_RANK + KV_LORA_RANK,
             POOL_WIDTH + Q_LORA_RANK + KV_LORA_RANK + QK_ROPE_DIM)
IN_COLS = IN_SPLITS[-1] + N_BRANCHES * D_MODEL
N_GROUPS = 4
EXPERTS_PER_GROUP = 8
N_EXPERTS = N_GROUPS * EXPERTS_PER_GROUP
TOP_K = 2
D_EXPERT = D_MODEL // 2
MOE_BLOCK = 128
NORM_EPS = 1e-5
DEEPNORM_ALPHA = (2.0 * DEPTH) ** 0.25
DEEPNORM_BETA = (8.0 * DEPTH) ** -0.25

kernel_name = "chunk_causal_pool_mla_hiermoe_deepnorm"


def layer_norm(x, g, b):
    xf = x.astype(jnp.float32)
    mu = jnp.mean(xf, axis=-1, keepdims=True)
    var = jnp.mean(jnp.square(xf - mu), axis=-1, keepdims=True)
    return ((xf - mu) * lax.rsqrt(var + NORM_EPS) * g.astype(jnp.float32) + b.astype(jnp.float32)).astype(x.dtype)


def rms_norm(x, g):
    xf = x.astype(jnp.float32)
    ms = jnp.mean(jnp.square(xf), axis=-1, keepdims=True)
    return (xf * lax.rsqrt(ms + NORM_EPS) * g.astype(jnp.float32)).astype(x.dtype)


def rope_tables(seq):
    inv = 1.0 / (ROPE_THETA ** (jnp.arange(0, QK_ROPE_DIM, 2, dtype=jnp.float32) / QK_ROPE_DIM))
    ang = jnp.arange(seq, dtype=jnp.float32)[:, None] * inv[None, :]
    return jnp.cos(ang), jnp.sin(ang)


def apply_rope(x, cos, sin):
    xf = x.astype(jnp.float32)
    x1, x2 = jnp.split(xf, 2, axis=-1)
    return jnp.concatenate([x1 * cos - x2 * sin, x1 * sin + x2 * cos], axis=-1).astype(x.dtype)


def multiscale_pool(u):
    B, S, _ = u.shape
    uf = u.astype(jnp.float32).reshape(B, S, POOL_GROUPS, POOL_GROUP_DIM)
    cs = jnp.pad(jnp.cumsum(uf, axis=1), ((0, 0), (1, 0), (0, 0), (0, 0)))
    t = jnp.arange(S)
    win = jnp.array(POOL_WINDOWS, dtype=jnp.int32)
    lo = jnp.maximum(t[:, None] + 1 - win[None, :], 0)
    gidx = jnp.arange(POOL_GROUPS)[None, :]
    win_sum = cs[:, 1:] - cs[:, lo, gidx, :]
    count = jnp.minimum(t[:, None] + 1, win[None, :]).astype(jnp.float32)
    return win_sum / count[None, :, :, None] - uf


def mla_branch(c_q, c_kv, k_pe_raw, q_norm_g, w_uq, kv_norm_g, w_ukv, w_mla_o, cos, sin):
    B, S, _ = c_q.shape
    q = (rms_norm(c_q, q_norm_g) @ w_uq).reshape(B, S, N_HEADS, QK_NOPE_DIM + QK_ROPE_DIM)
    q_nope, q_pe = q[..., :QK_NOPE_DIM], q[..., QK_NOPE_DIM:]
    q_pe = apply_rope(q_pe, cos[None, :, None, :], sin[None, :, None, :])
    kv = (rms_norm(c_kv, kv_norm_g) @ w_ukv).reshape(B, S, N_HEADS, QK_NOPE_DIM + V_HEAD_DIM)
    k_nope, v = kv[..., :QK_NOPE_DIM], kv[..., QK_NOPE_DIM:]
    k_pe = apply_rope(k_pe_raw, cos[None], sin[None])
    scale = (QK_NOPE_DIM + QK_ROPE_DIM) ** -0.5
    key_chunk = jnp.arange(S) // CHUNK

    def query_block(i):
        qs = i * Q_BLOCK
        qn = lax.dynamic_slice_in_dim(q_nope, qs, Q_BLOCK, axis=1)
        qp = lax.dynamic_slice_in_dim(q_pe, qs, Q_BLOCK, axis=1)
        s = (jnp.einsum('bqhd,bkhd->bhqk', qn, k_nope)
             + jnp.einsum('bqhd,bkd->bhqk', qp, k_pe)).astype(jnp.float32) * scale
        q_chunk = (qs + jnp.arange(Q_BLOCK)) // CHUNK
        mask = key_chunk[None, :] <= q_chunk[:, None]
        p = jax.nn.softmax(jnp.where(mask[None, None], s, -jnp.inf), axis=-1).astype(v.dtype)
        return jnp.einsum('bhqk,bkhd->bqhd', p, v)

    o = lax.map(query_block, jnp.arange(S // Q_BLOCK))
    o = jnp.transpose(o, (1, 0, 2, 3, 4)).reshape(B, S, N_HEADS * V_HEAD_DIM)
    return o @ w_mla_o


def hier_moe(h, w_rg, b_rg, w_re, b_re, w_gate, w_up, w_down):
    B, S, D = h.shape
    N = B * S
    t = h.reshape(N, D)
    g_prob = jax.nn.softmax((t @ w_rg).astype(jnp.float32) + b_rg.astype(jnp.float32), axis=-1)
    g_idx = jnp.argmax(g_prob, axis=-1)
    g_w = jnp.max(g_prob, axis=-1)
    e_logits = ((t @ w_re).astype(jnp.float32) + b_re.astype(jnp.float32)).reshape(N, N_GROUPS, EXPERTS_PER_GROUP)
    e_logits = jnp.take_along_axis(e_logits, g_idx[:, None, None], axis=1)[:, 0]
    top_p, top_i = lax.top_k(jax.nn.softmax(e_logits, axis=-1), TOP_K)
    gate = g_w[:, None] * top_p / jnp.sum(top_p, axis=-1, keepdims=True)
    expert = g_idx[:, None] * EXPERTS_PER_GROUP + top_i

    A = N * TOP_K
    n_blocks = -(-(A + N_EXPERTS * (MOE_BLOCK - 1)) // MOE_BLOCK)
    R = n_blocks * MOE_BLOCK
    flat_e = expert.reshape(A)
    flat_tok = jnp.repeat(jnp.arange(N), TOP_K)
    flat_w = gate.reshape(A)
    order = jnp.argsort(flat_e)
    sorted_e = flat_e[order]
    counts = jnp.bincount(flat_e, length=N_EXPERTS)
    padded = ((counts + MOE_BLOCK - 1) // MOE_BLOCK) * MOE_BLOCK
    pad_end = jnp.cumsum(padded)
    pad_start = pad_end - padded
    start = jnp.cumsum(counts) - counts
    dest = pad_start[sorted_e] + (jnp.arange(A) - start[sorted_e])
    row_tok = jnp.zeros((R,), jnp.int32).at[dest].set(flat_tok[order].astype(jnp.int32))
    row_w = jnp.zeros((R,), jnp.float32).at[dest].set(flat_w[order])
    block_e = jnp.minimum(jnp.searchsorted(pad_end, jnp.arange(n_blocks) * MOE_BLOCK, side='right'), N_EXPERTS - 1)

    def expert_block(args):
        tok, e = args
        xb = t[tok]
        hid = jax.nn.silu(xb @ w_gate[e]) * (xb @ w_up[e])
        return hid @ w_down[e]

    y = lax.map(expert_block, (row_tok.reshape(n_blocks, MOE_BLOCK), block_e)).reshape(R, D)
    out = jnp.zeros((N, D), jnp.float32).at[row_tok].add(y.astype(jnp.float32) * row_w[:, None])
    return out.astype(h.dtype).reshape(B, S, D)


def setup_inputs(seed: int = 0) -> dict:
    key = jax.random.key(seed)
    ks = jax.random.split(key, 24)
    L = DEPTH
    nrm = lambda k, shape, fan_in, s=1.0: jax.random.normal(k, shape, jnp.float32) * (s * fan_in ** -0.5)
    gain = lambda k, shape: 1.0 + 0.05 * jax.random.normal(k, shape, jnp.float32)
    small = lambda k, shape, s: s * jax.random.normal(k, shape, jnp.float32)
    return {
        "x": jax.random.normal(ks[0], (BATCH, SEQ, D_MODEL), jnp.float32),
        "w_in": nrm(ks[1], (L, D_MODEL, IN_COLS), D_MODEL),
        "pool_mix_w": nrm(ks[2], (L, POOL_GROUPS, POOL_GROUP_DIM, POOL_OUT_GROUP_DIM), POOL_GROUP_DIM),
        "pool_scale": gain(ks[3], (L, D_MODEL)),
        "q_norm_g": gain(ks[4], (L, Q_LORA_RANK)),
        "w_uq": nrm(ks[5], (L, Q_LORA_RANK, N_HEADS * (QK_NOPE_DIM + QK_ROPE_DIM)), Q_LORA_RANK),
        "kv_norm_g": gain(ks[6], (L, KV_LORA_RANK)),
        "w_ukv": nrm(ks[7], (L, KV_LORA_RANK, N_HEADS * (QK_NOPE_DIM + V_HEAD_DIM)), KV_LORA_RANK),
        "w_mla_o": nrm(ks[8], (L, N_HEADS * V_HEAD_DIM, D_MODEL), N_HEADS * V_HEAD_DIM),
        "w_out": nrm(ks[9], (L, D_MODEL, D_MODEL), D_MODEL, DEEPNORM_BETA),
        "ln1_g": gain(ks[10], (L, D_MODEL)),
        "ln1_b": small(ks[11], (L, D_MODEL), 0.02),
        "w_router_group": nrm(ks[12], (L, D_MODEL, N_GROUPS), D_MODEL),
        "b_router_group": small(ks[13], (L, N_GROUPS), 0.01),
        "w_router_expert": nrm(ks[14], (L, D_MODEL, N_EXPERTS), D_MODEL),
        "b_router_expert": small(ks[15], (L, N_EXPERTS), 0.01),
        "w_gate": nrm(ks[16], (L, N_EXPERTS, D_MODEL, D_EXPERT), D_MODEL),
        "w_up": nrm(ks[17], (L, N_EXPERTS, D_MODEL, D_EXPERT), D_MODEL),
        "w_down": nrm(ks[18], (L, N_EXPERTS, D_EXPERT, D_MODEL), D_EXPERT, DEEPNORM_BETA),
        "ln2_g": gain(ks[19], (L, D_MODEL)),
        "ln2_b": small(ks[20], (L, D_MODEL), 0.02),
    }


def reference(x, w_in, pool_mix_w, pool_scale, q_norm_g, w_uq, kv_norm_g, w_ukv, w_mla_o, w_out,
              ln1_g, ln1_b, w_router_group, b_router_group, w_router_expert, b_router_expert,
              w_gate, w_up, w_down, ln2_g, ln2_b):
    B, S, D = x.shape
    cos, sin = rope_tables(S)
    for l in range(DEPTH):
        proj = x @ w_in[l]
        u_pool, c_q, c_kv, k_pe_raw, gate_logits = jnp.split(proj, IN_SPLITS, axis=-1)
        pooled = multiscale_pool(u_pool).astype(x.dtype)
        y_pool = jnp.einsum('bsgc,gcd->bsgd', pooled, pool_mix_w[l]).reshape(B, S, D) * pool_scale[l]
        y_mla = mla_branch(c_q, c_kv, k_pe_raw, q_norm_g[l], w_uq[l], kv_norm_g[l], w_ukv[l],
                           w_mla_o[l], cos, sin)
        g = jax.nn.sigmoid(gate_logits.astype(jnp.float32)).reshape(B, S, N_BRANCHES, D)
        merged = (g[:, :, 0] * y_pool.astype(jnp.float32) + g[:, :, 1] * y_mla.astype(jnp.float32)).astype(x.dtype)
        x = layer_norm(DEEPNORM_ALPHA * x + merged @ w_out[l], ln1_g[l], ln1_b[l])
        moe = hier_moe(x, w_router_group[l], b_router_group[l], w_router_expert[l], b_router_expert[l],
                       w_gate[l], w_up[l], w_down[l])
        x = layer_norm(DEEPNORM_ALPHA * x + moe, ln2_g[l], ln2_b[l])
    return x
```

```python
import contextlib
import math
import numpy as np
import ml_dtypes
import concourse.bass as bass
import concourse.mybir as mybir
from concourse.alu_op_type import AluOpType as ALU
from concourse.bass_utils import run_bass_kernel_spmd

F32 = mybir.dt.float32
BF16 = mybir.dt.bfloat16
I32 = mybir.dt.int32
AF = mybir.ActivationFunctionType
AX = mybir.AxisListType

D = 1024
S = 8192
B = 4
NOWN = 4096
NT = 32
BLKR = 256
NBLK = 64
R = NBLK * BLKR
EPS = 1e-5
ALPHA = 2.0 ** 0.25
SCALE = 96.0 ** -0.5
DEBUG = False
STOP = ""
NHEADS_RUN = 8
NBLK_RUN = NBLK


class _Stop(Exception):
    pass


class Buf:
    __slots__ = ("name", "w", "r", "dsem", "dtot")

    def __init__(self, name):
        self.name = name
        self.w = None
        self.r = {}
        self.dsem = None
        self.dtot = 0


class Eng:
    def __init__(self, name, eng, sem):
        self.name, self.eng, self.sem, self.cnt, self.seen = name, eng, sem, 0, {}


class TK:
    def __init__(self, nc, es):
        self.nc, self.es = nc, es
        self.E = {}
        for n in ("tensor", "vector", "scalar", "gpsimd", "sync"):
            self.E[n] = Eng(n, getattr(nc, n), es.enter_context(nc.semaphore("es_" + n)))
        self.lanes = []
        self.bufs = []
        self.bar = es.enter_context(nc.semaphore("barrier"))
        self.barcnt = 0
        self.nb = 0

    def buf(self, name=None):
        self.nb += 1
        b = Buf(name or ("b%d" % self.nb))
        self.bufs.append(b)
        return b

    def _wait(self, e, sem, val):
        k = id(sem)
        if e.seen.get(k, 0) >= val:
            return
        e.eng.wait_ge(sem, val)
        e.seen[k] = val

    def _need(self, e, rec, kind):
        sem, val, owner = rec
        if owner is e and (e.name == "tensor" or kind == "war"):
            return
        self._wait(e, sem, val)

    def _deps(self, e, reads, writes):
        for b in reads:
            if b.w is not None:
                self._need(e, b.w, "raw")
        for b in writes:
            if b.w is not None:
                self._need(e, b.w, "waw")
            for rec in b.r.values():
                self._need(e, rec, "war")

    def op(self, en, fn, reads=(), writes=()):
        e = self.E[en]
        self._deps(e, reads, writes)
        ins = fn(e.eng)
        e.cnt += 1
        ins.then_inc(e.sem, 1)
        rec = (e.sem, e.cnt, e)
        for b in reads:
            b.r[id(e.sem)] = rec
        for b in writes:
            b.w = rec
            b.r = {}
        return ins

    def _lane(self, lane):
        if lane.dsem is None:
            lane.dsem = self.es.enter_context(self.nc.semaphore("ds%d" % len(self.lanes)))
            self.lanes.append(lane)

    def dma(self, qn, fn, reads=(), writes=(), lane=None):
        e = self.E[qn]
        self._deps(e, reads, writes)
        self._lane(lane)
        ins = fn(e.eng)
        lane.dtot += 16
        ins.then_inc(lane.dsem, 16)
        rec = (lane.dsem, lane.dtot, None)
        for b in reads:
            b.r[id(lane.dsem)] = rec
        for b in writes:
            b.w = rec
            b.r = {}
        return ins

    def barrier(self):
        sy = self.E["sync"]
        for e in self.E.values():
            if e is not sy and e.cnt > 0:
                self._wait(sy, e.sem, e.cnt)
        for l in self.lanes:
            if l.dtot > 0:
                self._wait(sy, l.dsem, l.dtot)
        self.barcnt += 1
        sy.eng.sem_inc(self.bar, 1)
        for e in self.E.values():
            if e is not sy:
                e.eng.wait_ge(self.bar, self.barcnt)
        for b in self.bufs:
            b.w = None
            b.r = {}
        for e in self.E.values():
            for e2 in self.E.values():
                e.seen[id(e2.sem)] = e2.cnt
            for l in self.lanes:
                e.seen[id(l.dsem)] = l.dtot


def build_program():
    nc = bass.Bass("TRN2", target_bir_lowering=False)

    def din(name, shape, dt=F32):
        return nc.dram_tensor(name, list(shape), dt, kind="ExternalInput").ap()

    def dscr(name, shape, dt, dbg=False):
        kind = "ExternalOutput" if (dbg and DEBUG) else "Internal"
        return nc.dram_tensor(name, list(shape), dt, kind=kind).ap()

    xT_all = din("xT_all", [128, 8, S])
    xT_own = din("xT_own", [128, 8, NOWN])
    x_own = din("x_own", [NOWN, D])
    w_pool_d = din("w_pool", [128, 8, 512])
    w_cq_d = din("w_cq", [128, 8, 384])
    w_ckv_d = din("w_ckv", [128, 8, 256])
    w_kpe_d = din("w_kpe2", [128, 8, 64])
    w_kpesw_d = din("w_kpesw2", [128, 8, 64])
    w_gates_d = din("w_gates", [128, 8, 2048])
    w_uq_d = din("w_uq_l", [128, 3, 8, 128])
    qg_d = din("qg", [128, 3])
    w_uk_d = din("w_uk_l", [128, 2, 8, 64])
    w_uv_d = din("w_uv_l", [128, 2, 8, 64])
    kvg_d = din("kvg", [128, 2])
    w_mo_d = din("w_mo_l", [128, 4, 1024])
    w_out_d = din("w_out_l", [128, 8, 1024])
    w_pm_d = din("w_pm_l", [128, 4, 256])
    psc_d = din("psc", [128, 8])
    ln1g_d = din("ln1_g", [1, D])
    ln1b_d = din("ln1_b", [1, D])
    ln2g_d = din("ln2_g", [1, D])
    ln2b_d = din("ln2_b", [1, D])
    w_r_d = din("w_r", [128, 8, 36])
    b_r_d = din("b_r", [1, 36])
    wg_d = din("wg_l", [32 * 128, 4096])
    wu_d = din("wu_l", [32 * 128, 4096])
    wd_d = din("wd_l", [32 * 128, 4096])
    tblq_d = din("tblq", [128, NOWN])
    cosk_d = din("cosk", [64, S])
    sink_d = din("sink", [64, S])
    amain_d = din("a_main", [128, 4, 64])
    ahalo_d = din("a_halo", [128, 4, 64])
    afirst_d = din("a_first", [128, 4, 64])
    ident_d = din("ident", [128, 128])
    lstrict_d = din("lstrict", [128, 128])

    out_d = nc.dram_tensor("out", [NOWN, D], F32, kind="ExternalOutput").ap()
    poolT_d = dscr("poolT_s", [128, 4, NOWN], BF16)
    h1_d = dscr("h1_s", [NOWN, D], F32, dbg=True)
    xs_d = dscr("xs_s", [R, D], BF16)
    ys_d = dscr("ys_s", [R, D], F32)
    w16_all = dscr("w16_all", [32 * 128, 3 * 4096], BF16)
    wC16 = {"gates": dscr("wc_gates", [128, 8, 2048], BF16), "mo": dscr("wc_mo", [128, 4, 1024], BF16),
            "out": dscr("wc_out", [128, 8, 1024], BF16), "pm": dscr("wc_pm", [128, 4, 256], BF16)}
    if DEBUG:
        d_ckvn = dscr("d_ckvn", [128, 2, S], BF16, dbg=True)
        d_cqn = dscr("d_cqn", [128, 3, NOWN], BF16, dbg=True)
        d_kt = dscr("d_kt", [128, S], BF16, dbg=True)
        d_ot = dscr("d_ot", [128, 4, NOWN], BF16, dbg=True)
        d_lg = dscr("d_lg", [128, NT, 36], F32, dbg=True)
        d_rt = dscr("d_rt", [128, 4, NT], F32, dbg=True)
        d_idx = dscr("d_idx", [128, NBLK], I32, dbg=True)

    TREF = []

    def _body(es):
        T = TK(nc, es)
        TREF.append(T)
        op, dma = T.op, T.dma

        def sbt(ctx, name, shape, dt):
            return ctx.enter_context(nc.sbuf_tensor("s_" + name, list(shape), dt))

        def pst(ctx, name, shape, dt):
            return ctx.enter_context(nc.psum_tensor("p_" + name, list(shape), dt))

        def mm(out, lhsT, rhs, start, stop, reads, writes):
            return op("tensor", lambda e: e.matmul(out, lhsT=lhsT, rhs=rhs, start=start, stop=stop),
                      reads=reads, writes=writes)

        OT = sbt(es, "OT", [128, 4, NOWN], BF16)
        B_OT = [T.buf("OT%d" % i) for i in range(4)]
        D1i = sbt(es, "D1i", [128, NT], I32)
        D2i = sbt(es, "D2i", [128, NT], I32)
        G1 = sbt(es, "G1", [128, NT], F32)
        G2 = sbt(es, "G2", [128, NT], F32)
        IDXW = sbt(es, "IDXW", [128, NBLK], I32)
        B_rt = T.buf("route")
        LG = sbt(es, "LG", [128, NT, 36], F32)
        B_LG = T.buf("LG")
        identf = sbt(es, "identf", [128, 128], F32)
        identb = sbt(es, "identb", [128, 128], BF16)
        onesb = sbt(es, "onesb", [128, 128], BF16)
        B_const = T.buf("const")
        dma("sync", lambda e: e.dma_start(out=identf[:], in_=ident_d[:, :]), writes=[B_const], lane=B_const)
        dma("gpsimd", lambda e: e.dma_start(out=identb[:], in_=ident_d[:, :]), writes=[B_const], lane=B_const)
        op("vector", lambda e: e.memset(onesb[:], 1.0), writes=[B_const])

        with contextlib.ExitStack() as cAB:
            CKVN = sbt(cAB, "CKVN", [128, 2, S], BF16)
            CQN = sbt(cAB, "CQN", [128, 3, NOWN], BF16)
            KT = [sbt(cAB, "KT%d" % i, [128, S], BF16) for i in range(2)]
            B_ckvn, B_cqn = T.buf("ckvn"), T.buf("cqn")
            B_KTlo = [T.buf("KTlo0"), T.buf("KTlo1")]
            B_KThi = [T.buf("KThi0"), T.buf("KThi1")]

            with contextlib.ExitStack() as cA:
                w_ckv = sbt(cA, "w_ckv", [128, 8, 256], BF16)
                w_kpe = sbt(cA, "w_kpe", [128, 8, 64], BF16)
                w_kpesw = sbt(cA, "w_kpesw", [128, 8, 64], BF16)
                w_pool = sbt(cA, "w_pool", [128, 8, 512], BF16)
                w_cq = sbt(cA, "w_cq", [128, 8, 384], BF16)
                a_main = sbt(cA, "a_main", [128, 4, 64], BF16)
                a_halo = sbt(cA, "a_halo", [128, 4, 64], BF16)
                a_first = sbt(cA, "a_first", [128, 4, 64], BF16)
                BW = {}
                for nm_, t_, d_ in (("ckv", w_ckv, w_ckv_d), ("kpe", w_kpe, w_kpe_d), ("kpesw", w_kpesw, w_kpesw_d),
                                    ("pool", w_pool, w_pool_d), ("am", a_main, amain_d), ("ah", a_halo, ahalo_d),
                                    ("af", a_first, afirst_d), ("cq", w_cq, w_cq_d)):
                    BW[nm_] = T.buf("wA_" + nm_)
                    dma("gpsimd", lambda e, t_=t_, d_=d_: e.dma_start(out=t_[:], in_=d_[:, :, :]),
                        writes=[BW[nm_]], lane=BW[nm_])
                xb = [sbt(cA, "xb%d" % i, [128, 8, 512], BF16) for i in range(2)]
                B_xb = [T.buf("xb0"), T.buf("xb1")]
                cosb = [sbt(cA, "cosb%d" % i, [64, 512], F32) for i in range(2)]
                sinb = [sbt(cA, "sinb%d" % i, [64, 512], F32) for i in range(2)]
                B_cs = [T.buf("cs0"), T.buf("cs1")]
                sq = sbt(cA, "sq", [128, 3, 512], BF16)
                B_sq = [T.buf("sq0"), T.buf("sq1"), T.buf("sq2")]
                rs = sbt(cA, "rs", [128, 512], F32)
                B_rs = T.buf("rs")
                t1 = sbt(cA, "t1", [64, 512], F32)
                t2 = sbt(cA, "t2", [64, 512], F32)
                B_t1, B_t2 = T.buf("t1"), T.buf("t2")
                UT = [sbt(cA, "UT%d" % i, [128, 512], BF16) for i in range(6)]
                B_UT = [T.buf("UT%d" % i) for i in range(6)]
                PL = [sbt(cA, "PL%d" % i, [128, 4, 256], BF16) for i in range(2)]
                B_PL = [T.buf("PL0"), T.buf("PL1")]
                P = [pst(cA, "PA%d" % i, [128, 512], F32) for i in range(8)]
                B_P = [T.buf("PA%d" % i) for i in range(8)]

                def load_all(tb):
                    s = tb % 2
                    dma("gpsimd", lambda e: e.dma_start(out=xb[s][:], in_=xT_all[:, :, tb * 512:(tb + 1) * 512]),
                        writes=[B_xb[s]], lane=B_xb[s])
                    dma("sync", lambda e: e.dma_start(out=cosb[s][:], in_=cosk_d[:, tb * 512:(tb + 1) * 512]),
                        writes=[B_cs[s]], lane=B_cs[s])
                    dma("sync", lambda e: e.dma_start(out=sinb[s][:], in_=sink_d[:, tb * 512:(tb + 1) * 512]),
                        writes=[B_cs[s]], lane=B_cs[s])

                def norm_block(nm, wt, xs_, Bx, dest, B_dest, col0, nfeat):
                    Bw_ = BW["ckv"] if nm == 2 else BW["cq"]
                    for m in range(nm):
                        for c in range(8):
                            mm(P[m][:], wt[:, c, m * 128:(m + 1) * 128], xs_[:, c, :], c == 0, c == 7,
                               [Bw_, Bx], [B_P[m]])
                    for m in range(nm):
                        op("scalar", lambda e: e.activation(out=sq[:, m, :], in_=P[m][:], func=AF.Square),
                           reads=[B_P[m]], writes=[B_sq[m]])
                    return

                def norm_finish(nm, dest, B_dest, col0, nfeat):
                    for m in range(nm):
                        mm(P[3][:], onesb[:], sq[:, m, :], m == 0, m == nm - 1, [B_const, B_sq[m]], [B_P[3]])
                    op("scalar", lambda e: e.activation(out=rs[:], in_=P[3][:], func=AF.Sqrt,
                                                         scale=1.0 / nfeat, bias=EPS),
                       reads=[B_P[3]], writes=[B_rs])
                    op("vector", lambda e: e.reciprocal(out=rs[:], in_=rs[:]), reads=[B_rs], writes=[B_rs])
                    for m in range(nm):
                        op("vector", lambda e: e.tensor_tensor(out=dest[:, m, col0:col0 + 512], in0=P[m][:],
                                                               in1=rs[:], op=ALU.mult),
                           reads=[B_P[m], B_rs], writes=[B_dest])

                load_all(0)
                for tb in range(16):
                    s = tb % 2
                    if tb + 1 < 16:
                        load_all(tb + 1)
                    xs_ = xb[s]
                    c0 = tb * 512
                    norm_block(2, w_ckv, xs_, B_xb[s], CKVN, B_ckvn, c0, 256)
                    for (pi, wt, bn_) in ((4, w_kpe, "kpe"), (5, w_kpesw, "kpesw")):
                        for c in range(8):
                            mm(P[pi][0:64, :], wt[:, c, :], xs_[:, c, :], c == 0, c == 7, [BW[bn_], B_xb[s]], [B_P[pi]])
                    norm_finish(2, CKVN, B_ckvn, c0, 256.0)
                    op("vector", lambda e: e.tensor_tensor(out=t1[:], in0=P[4][0:64, :], in1=cosb[s][:], op=ALU.mult),
                       reads=[B_P[4], B_cs[s]], writes=[B_t1])
                    op("vector", lambda e: e.tensor_tensor(out=t2[:], in0=P[5][0:64, :], in1=sinb[s][:], op=ALU.mult),
                       reads=[B_P[5], B_cs[s]], writes=[B_t2])
                    op("gpsimd", lambda e: e.tensor_tensor(out=KT[0][0:64, c0:c0 + 512], in0=t1[:], in1=t2[:], op=ALU.add),
                       reads=[B_t1, B_t2], writes=[B_KTlo[0]])
                    op("gpsimd", lambda e: e.tensor_tensor(out=KT[1][0:64, c0:c0 + 512], in0=t1[:], in1=t2[:], op=ALU.add),
                       reads=[B_t1, B_t2], writes=[B_KTlo[1]])
                    for tt in range(4):
                        G = tb * 4 + tt
                        pu = 6 + (tt % 2)
                        for c in range(8):
                            mm(P[pu][:], xs_[:, c, tt * 128:(tt + 1) * 128], w_pool[:, c, :], c == 0, c == 7,
                               [BW["pool"], B_xb[s]], [B_P[pu]])
                        op("scalar", lambda e: e.activation(out=UT[G % 6][:], in_=P[pu][:], func=AF.Copy),
                           reads=[B_P[pu]], writes=[B_UT[G % 6]])
                    for tt in range(4):
                        G = tb * 4 + tt
                        pp = P[2][:, 0:256].rearrange("p (g j) -> p g j", g=4)
                        for g in range(4):
                            am = a_first if G == 0 else a_main
                            mm(pp[:, g, :], UT[G % 6][:, g * 128:(g + 1) * 128], am[:, g, :], True, G == 0,
                               [B_UT[G % 6], BW["af"], BW["am"]], [B_P[2]])
                            if G > 0:
                                mm(pp[:, g, :], UT[(G - 1) % 6][64:128, g * 128:(g + 1) * 128], a_halo[64:128, g, :],
                                   False, True, [B_UT[(G - 1) % 6], BW["ah"]], [B_P[2]])
                        op("vector", lambda e: e.tensor_copy(out=PL[s][:, :, tt * 64:(tt + 1) * 64], in_=pp),
                           reads=[B_P[2]], writes=[B_PL[s]])
                    dma("sync", lambda e: e.dma_start(out=poolT_d[:, :, tb * 256:(tb + 1) * 256], in_=PL[s][:]),
                        reads=[B_PL[s]], lane=B_PL[s])

                def load_own(qb):
                    s = qb % 2
                    dma("gpsimd", lambda e: e.dma_start(out=xb[s][:], in_=xT_own[:, :, qb * 512:(qb + 1) * 512]),
                        writes=[B_xb[s]], lane=B_xb[s])
                load_own(0)
                for qb in range(8):
                    s = qb % 2
                    if qb + 1 < 8:
                        load_own(qb + 1)
                    norm_block(3, w_cq, xb[s], B_xb[s], CQN, B_cqn, qb * 512, 384)
                    norm_finish(3, CQN, B_cqn, qb * 512, 384.0)
                if DEBUG:
                    dma("sync", lambda e: e.dma_start(out=d_ckvn[:, :, :], in_=CKVN[:]), reads=[B_ckvn], lane=B_ckvn)
                    dma("sync", lambda e: e.dma_start(out=d_cqn[:, :, :], in_=CQN[:]), reads=[B_cqn], lane=B_cqn)
                T.barrier()
                if STOP == "A":
                    return

            with contextlib.ExitStack() as cB:
                w_uq = sbt(cB, "w_uq", [128, 3, 8, 128], BF16)
                w_uk = sbt(cB, "w_uk", [128, 2, 8, 64], BF16)
                w_uv = sbt(cB, "w_uv", [128, 2, 8, 64], BF16)
                qg = sbt(cB, "qg", [128, 3], F32)
                kvg = sbt(cB, "kvg", [128, 2], F32)
                B_wB, B_st = T.buf("wB"), T.buf("stB")
                cSt = contextlib.ExitStack()
                stage = sbt(cSt, "stageB", [128, 3, 8, 128], F32)
                dma("sync", lambda e: e.dma_start(out=qg[:], in_=qg_d[:, :]), writes=[B_wB], lane=B_wB)
                dma("sync", lambda e: e.dma_start(out=kvg[:], in_=kvg_d[:, :]), writes=[B_wB], lane=B_wB)
                dma("sync", lambda e: e.dma_start(out=stage[:], in_=w_uq_d[:, :, :, :]), writes=[B_st], lane=B_st)
                for kc in range(3):
                    op("vector", lambda e: e.tensor_scalar_mul(out=w_uq[:, kc, :, :], in0=stage[:, kc, :, :],
                                                               scalar1=qg[:, kc:kc + 1]),
                       reads=[B_st, B_wB], writes=[B_wB])
                for (wt, wd_) in ((w_uk, w_uk_d), (w_uv, w_uv_d)):
                    stv = stage[:, 0:2, :, 0:64]
                    dma("sync", lambda e: e.dma_start(out=stv, in_=wd_[:, :, :, :]), writes=[B_st], lane=B_st)
                    for m in range(2):
                        op("vector", lambda e: e.tensor_scalar_mul(out=wt[:, m, :, :], in0=stage[:, m, :, 0:64],
                                                                   scalar1=kvg[:, m:m + 1]),
                           reads=[B_st, B_wB], writes=[B_wB])
                T.barrier()
                cSt.close()
                B_wconv = T.buf("wconv")
                for nm_, src_ in (("gates", w_gates_d), ("mo", w_mo_d), ("out", w_out_d), ("pm", w_pm_d)):
                    dma("gpsimd", lambda e: e.dma_start(out=wC16[nm_][:, :, :], in_=src_[:, :, :]), lane=B_wconv)
                for wi, wsrc in enumerate((wg_d, wu_d, wd_d)):
                    for r8 in range(8):
                        o_ = w16_all[r8 * 512:(r8 + 1) * 512, wi * 4096:(wi + 1) * 4096].rearrange("r (a n) -> r a n", n=2048)
                        i_ = wsrc[r8 * 512:(r8 + 1) * 512, :].rearrange("r (a n) -> r a n", n=2048)
                        dma("gpsimd", lambda e: e.dma_start(out=o_, in_=i_), lane=B_wconv)
                VT = [sbt(cB, "VT%d" % i, [128, 64, 128], BF16) for i in range(2)]
                B_VT = [T.buf("VT0"), T.buf("VT1")]
                op("gpsimd", lambda e: e.memset(VT[0][:, :, 64:128], 1.0), writes=[B_VT[0]])
                op("gpsimd", lambda e: e.memset(VT[1][:, :, 0:64], 1.0), writes=[B_VT[1]])
                QT = [sbt(cB, "QT%d" % i, [128, 512], BF16) for i in range(2)]
                B_QT = [T.buf("QT0"), T.buf("QT1")]
                tq = [sbt(cB, "tq%d" % i, [128, 512], F32) for i in range(2)]
                B_tq = [T.buf("tq0"), T.buf("tq1")]
                NPT = 3
                PT2 = [sbt(cB, "PT2_%d" % i, [128, 2, 512], BF16) for i in range(NPT)]
                B_PT2 = [T.buf("PT2_%d" % i) for i in range(NPT)]
                rden = sbt(cB, "rden", [128, 512], F32)
                B_rden = T.buf("rden")
                PS2 = [pst(cB, "PS2_%d" % i, [128, 2, 512], F32) for i in range(2)]
                B_PS2h = [[T.buf("PS2_%d_%d" % (i, hh)) for hh in range(2)] for i in range(2)]
                P = {i: pst(cB, "PB%d" % i, [128, 512], F32) for i in (3, 4, 5, 6)}
                B_P = {i: T.buf("PB%d" % i) for i in (3, 4, 5, 6)}
                brot = [0]
                bmode = ["all"]
                BUILD_VIEWS = [(PS2[0][:, 0, :], B_PS2h[0][0]), (PS2[0][:, 1, :], B_PS2h[0][1]),
                               (PS2[1][:, 0, :], B_PS2h[1][0]), (PS2[1][:, 1, :], B_PS2h[1][1]),
                               (P[6][:], B_P[6])]

                def build_kv_items(h):
                    items = []
                    for kb in range(16):
                        items.append(lambda kb=kb: build_k_item(h, kb))
                    for grp in range(8):
                        items.append(lambda grp=grp: build_v_item(h, grp))
                    return items

                def build_k_item(h, kb):
                    kb_ = h % 2
                    if True:
                        pv_, Bp = BUILD_VIEWS[brot[0] % 5] if bmode[0] == "all" else BUILD_VIEWS[4]
                        brot[0] += 1
                        for m in range(2):
                            mm(pv_[64:128, :], w_uk[:, m, h, :], CKVN[:, m, kb * 512:(kb + 1) * 512], m == 0, m == 1,
                               [B_wB, B_ckvn], [Bp])
                        if False:
                            pass
                        else:
                            op("vector", lambda e: e.tensor_copy(out=KT[kb_][64:128, kb * 512:(kb + 1) * 512],
                                                                 in_=pv_[64:128, :]),
                               reads=[Bp], writes=[B_KThi[kb_]])

                def build_v_item(h, grp):
                    kb_ = h % 2
                    voff = 0 if h % 2 == 0 else 64
                    if True:
                        pv_, Bp = BUILD_VIEWS[brot[0] % 5] if bmode[0] == "all" else BUILD_VIEWS[4]
                        brot[0] += 1
                        pv = pv_.rearrange("p (j v) -> p j v", j=8)
                        for j in range(8):
                            kt = grp * 8 + j
                            for m in range(2):
                                mm(pv[:, j, :], CKVN[:, m, kt * 128:(kt + 1) * 128], w_uv[:, m, h, :], m == 0, m == 1,
                                   [B_wB, B_ckvn], [Bp])
                        if True:
                            op("vector", lambda e: e.tensor_copy(out=VT[kb_][:, grp * 8:(grp + 1) * 8, voff:voff + 64],
                                                                 in_=pv),
                               reads=[Bp], writes=[B_VT[kb_]])

                def build_q(step):
                    h, Q = step // 8, step % 8
                    s = step % 2
                    dma("sync", lambda e: e.dma_start(out=tq[s][:], in_=tblq_d[:, Q * 512:(Q + 1) * 512]),
                        writes=[B_tq[s]], lane=B_tq[s])
                    for kc in range(3):
                        mm(P[5][:], w_uq[:, kc, h, :], CQN[:, kc, Q * 512:(Q + 1) * 512], kc == 0, kc == 2,
                           [B_wB, B_cqn], [B_P[5]])
                    op("vector", lambda e: e.tensor_tensor(out=QT[s][:], in0=P[5][:], in1=tq[s][:], op=ALU.mult),
                       reads=[B_P[5], B_tq[s]], writes=[B_QT[s]])

                unit_ctr = [0]

                def attn_step(step, fill):
                    h, Q = step // 8, step % 8
                    s = step % 2
                    kb_ = h % 2
                    po = 3 + (step % 2)
                    pair = h // 2
                    nfull = 8 * Q
                    units = []
                    for u in range(4 * Q):
                        units.append((False, ((2 * u, 0), (2 * u + 1, 0))))
                    for jp in range(4):
                        je, jo = 2 * jp, 2 * jp + 1
                        units.append((True, ((nfull + je, 64 * je), (nfull + jo, 64 * jo))))
                    total_pv = 2 * len(units)
                    if h % 2 == 0:
                        nlo, dlo = 0, 64
                    else:
                        nlo, dlo = 64, 0

                    def finalize():
                        op("vector", lambda e: e.reciprocal(out=rden[nlo:nlo + 64, :], in_=P[po][dlo:dlo + 64, :]),
                           reads=[B_P[po]], writes=[B_rden])
                        op("vector", lambda e: e.tensor_tensor(out=OT[nlo:nlo + 64, pair, Q * 512:(Q + 1) * 512],
                                                               in0=P[po][nlo:nlo + 64, :], in1=rden[nlo:nlo + 64, :],
                                                               op=ALU.mult),
                           reads=[B_P[po], B_rden], writes=[B_OT[pair]])

                    ctx = {"npv": 0, "total": total_pv, "po": po, "kb": kb_, "fin": finalize}

                    def emit_qk(unit):
                        diag, tl = unit
                        ui = unit_ctr[0]
                        unit_ctr[0] += 1
                        ps, pt = ui % 2, ui % NPT
                        c0 = tl[0][1]
                        for hh, (k, c) in enumerate(tl):
                            mm(PS2[ps][:, hh, c:512], KT[kb_][:, k * 128:(k + 1) * 128], QT[s][:, c:512], True, True,
                               [B_KTlo[kb_], B_KThi[kb_], B_QT[s]], [B_PS2h[ps][hh]])
                        op("scalar", lambda e: e.activation(out=PT2[pt][:, :, c0:512], in_=PS2[ps][:, :, c0:512],
                                                            func=AF.Exp),
                           reads=[B_PS2h[ps][0], B_PS2h[ps][1]], writes=[B_PT2[pt]])
                        if diag:
                            for hh, (k, c) in enumerate(tl):
                                op("gpsimd", lambda e: e.memset(PT2[pt][64:128, hh, c:c + 32], 0.0),
                                   writes=[B_PT2[pt]])
                        return (tl, pt, ctx)

                    for ui_, unit in enumerate(units):
                        GP.append(emit_qk(unit))
                        if len(GP) > 2:
                            emit_pv(GP.pop(0))
                        if fill and ui_ % 6 == 5:
                            fill.pop(0)()

                GP = []

                def emit_pv(rec):
                    tl, pt, ctx = rec
                    po_, kbx = ctx["po"], ctx["kb"]
                    for hh, (k, c) in enumerate(tl):
                        ctx["npv"] += 1
                        mm(P[po_][:, c:512], VT[kbx][:, k, :], PT2[pt][:, hh, c:512], ctx["npv"] == 1,
                           ctx["npv"] == ctx["total"], [B_VT[kbx], B_PT2[pt]], [B_P[po_]])
                    if ctx["npv"] == ctx["total"]:
                        ctx["fin"]()

                for it in build_kv_items(0):
                    it()
                bmode[0] = "fill"
                build_q(0)
                NH = NHEADS_RUN
                for h in range(NH):
                    fill = build_kv_items(h + 1) if h + 1 < NH else []
                    for Q in range(8):
                        step = h * 8 + Q
                        if step + 1 < NH * 8:
                            build_q(step + 1)
                        attn_step(step, fill)
                    while fill:
                        fill.pop(0)()
                while GP:
                    emit_pv(GP.pop(0))
                if DEBUG:
                    dma("sync", lambda e: e.dma_start(out=d_kt[:, :], in_=KT[1][:]), reads=[B_KTlo[1], B_KThi[1]],
                        lane=B_KTlo[1])
                    dma("sync", lambda e: e.dma_start(out=d_ot[:, :, :], in_=OT[:]), reads=B_OT, lane=B_OT[0])
                T.barrier()
                if STOP == "B":
                    return

        with contextlib.ExitStack() as cC:
            w_gates = sbt(cC, "w_gates", [128, 8, 2048], BF16)
            w_mo = sbt(cC, "w_mo", [128, 4, 1024], BF16)
            w_out = sbt(cC, "w_out", [128, 8, 1024], BF16)
            w_pm = sbt(cC, "w_pm", [128, 4, 256], BF16)
            psc = sbt(cC, "psc", [128, 8], F32)
            lng = sbt(cC, "lng", [128, D], F32)
            lnb = sbt(cC, "lnb", [128, D], F32)
            w_r = sbt(cC, "w_r", [128, 8, 36], F32)
            brb = sbt(cC, "brb", [128, 36], F32)
            B_wC = T.buf("wC")
            B_wG = T.buf("wG")
            dma("sync", lambda e: e.dma_start(out=w_gates[:], in_=wC16["gates"][:, :, :]), writes=[B_wG], lane=B_wG)
            for t_, d_ in ((w_mo, wC16["mo"]), (w_out, wC16["out"]), (w_pm, wC16["pm"])):
                dma("sync", lambda e, t_=t_, d_=d_: e.dma_start(out=t_[:], in_=d_[:, :, :]), writes=[B_wC], lane=B_wC)
            dma("sync", lambda e: e.dma_start(out=psc[:], in_=psc_d[:, :]), writes=[B_wC], lane=B_wC)
            dma("sync", lambda e: e.dma_start(out=lng[:], in_=ln1g_d.broadcast_to([128, D])), writes=[B_wC], lane=B_wC)
            dma("sync", lambda e: e.dma_start(out=lnb[:], in_=ln1b_d.broadcast_to([128, D])), writes=[B_wC], lane=B_wC)
            dma("sync", lambda e: e.dma_start(out=w_r[:], in_=w_r_d[:, :, :]), writes=[B_wC], lane=B_wC)
            dma("sync", lambda e: e.dma_start(out=brb[:], in_=b_r_d.broadcast_to([128, 36])), writes=[B_wC], lane=B_wC)
            _xbo = sbt(cC, "xbo", [128, 8, 512], BF16)
            xbo = [_xbo, _xbo]
            _bx = T.buf("xbo")
            B_xbo = [_bx, _bx]
            _plb = sbt(cC, "PLb", [128, 4, 512], BF16)
            PLb = [_plb, _plb]
            _bp = T.buf("PLb")
            B_PLb = [_bp, _bp]
            Gt = sbt(cC, "Gt", [128, 16, 512], BF16)
            B_G = [T.buf("G%d" % i) for i in range(16)]
            MT = [sbt(cC, "MT%d" % i, [128, 8, 512], BF16) for i in range(2)]
            B_MT = [T.buf("MT0"), T.buf("MT1")]
            At2 = [sbt(cC, "At%d" % i, [128, 512], F32) for i in range(2)]
            Bt2 = [sbt(cC, "Bt%d" % i, [128, 512], F32) for i in range(2)]
            B_At2 = [T.buf("At0"), T.buf("At1")]
            B_Bt2 = [T.buf("Bt0"), T.buf("Bt1")]
            mhalf = sbt(cC, "mhalf", [128, 1], F32)
            B_mh = T.buf("mhalf")
            op("gpsimd", lambda e: e.memset(mhalf[:], -0.5), writes=[B_mh])
            xo = [sbt(cC, "xo%d" % i, [128, D], F32) for i in range(2)]
            B_xo = [T.buf("xo0"), T.buf("xo1")]
            R1s = [sbt(cC, "R1_%d" % i, [128, D], F32) for i in range(2)]
            B_R1s = [T.buf("R1_0"), T.buf("R1_1")]
            H1 = [sbt(cC, "H1%d" % i, [128, D], F32) for i in range(3)]
            B_H1 = [T.buf("H10"), T.buf("H11"), T.buf("H12")]
            H1T = sbt(cC, "H1T", [128, 8, 128], F32)
            B_H1T = [T.buf("H1Ta"), T.buf("H1Tb")]
            P = [pst(cC, "PC%d" % i, [128, 512], F32) for i in range(8)]
            B_P = [T.buf("PC%d" % i) for i in range(8)]

            LNT = {}
            for nm_ in ("st", "mv", "rstd", "nbias", "veps"):
                shp = {"st": [128, 2, 6], "mv": [128, 2], "rstd": [128, 1], "nbias": [128, 1], "veps": [128, 1]}[nm_]
                LNT[nm_] = [sbt(cC, "ln_%s%d" % (nm_, i), shp, F32) for i in range(2)]
                LNT["B_" + nm_] = [T.buf("ln_%s%d" % (nm_, i)) for i in range(2)]

            def ln_p1a(i, src, B_src, B_gb):
                k = i % 2
                st, mv, rstd, veps = LNT["st"][k], LNT["mv"][k], LNT["rstd"][k], LNT["veps"][k]
                B_st, B_mv, B_rstd, B_veps = LNT["B_st"][k], LNT["B_mv"][k], LNT["B_rstd"][k], LNT["B_veps"][k]
                for c in range(2):
                    op("vector", lambda e: e.bn_stats(out=st[:, c, :], in_=src[:, c * 512:(c + 1) * 512]),
                       reads=[B_src], writes=[B_st])
                op("vector", lambda e: e.bn_aggr(out=mv[:, :], in_=st[:, :, :].rearrange("p a b -> p (a b)")),
                   reads=[B_st], writes=[B_mv])
                op("gpsimd", lambda e: e.tensor_scalar(out=veps[:], in0=mv[:, 1:2], scalar1=EPS, scalar2=None, op0=ALU.add),
                   reads=[B_mv], writes=[B_veps])
                op("gpsimd", lambda e: e.tensor_tensor(out=rstd[:], in0=veps[:], in1=mhalf[:], op=ALU.pow),
                   reads=[B_veps, B_mh], writes=[B_rstd])

            def ln_p1b(i, src, B_src, dst, B_dst):
                k = i % 2
                mv, rstd, nbias = LNT["mv"][k], LNT["rstd"][k], LNT["nbias"][k]
                B_mv, B_rstd, B_nbias = LNT["B_mv"][k], LNT["B_rstd"][k], LNT["B_nbias"][k]
                op("vector", lambda e: e.scalar_tensor_tensor(out=nbias[:], in0=mv[:, 0:1], scalar=-1.0, in1=rstd[:],
                                                              op0=ALU.mult, op1=ALU.mult),
                   reads=[B_mv, B_rstd], writes=[B_nbias])
                op("scalar", lambda e: e.activation(out=dst[:], in_=src[:], func=AF.Identity, scale=rstd[:, 0:1],
                                                    bias=nbias[:, 0:1]),
                   reads=[B_src, B_nbias, B_rstd], writes=[B_dst])

            def ln_p2(dst, B_dst, g_t, b_t, B_gb):
                op("vector", lambda e: e.tensor_tensor(out=dst[:], in0=dst[:], in1=g_t[:], op=ALU.mult),
                   reads=[B_dst, B_gb], writes=[B_dst])
                op("vector", lambda e: e.tensor_tensor(out=dst[:], in0=dst[:], in1=b_t[:], op=ALU.add),
                   reads=[B_dst, B_gb], writes=[B_dst])

            def loadC_x(Q):
                dma("gpsimd", lambda e: e.dma_start(out=xbo[0][:], in_=xT_own[:, :, Q * 512:(Q + 1) * 512]),
                    writes=[B_xbo[0]], lane=B_xbo[0])

            def loadC_p(Q):
                dma("sync", lambda e: e.dma_start(out=PLb[0][:], in_=poolT_d[:, :, Q * 512:(Q + 1) * 512]),
                    writes=[B_PLb[0]], lane=B_PLb[0])

            def stage1a(i, ms):
                tt = i % 4
                hs = i % 2
                if i == 0:
                    dma("sync", lambda e: e.dma_start(out=xo[0][:], in_=x_own[0:128, :]),
                        writes=[B_xo[0]], lane=B_xo[0])
                if i + 1 < NT:
                    hn = (i + 1) % 2
                    dma("sync", lambda e: e.dma_start(out=xo[hn][:], in_=x_own[(i + 1) * 128:(i + 2) * 128, :]),
                        writes=[B_xo[hn]], lane=B_xo[hn])
                for h2 in range(2):
                    for dc in range(8):
                        mm(P[4 + h2][:], MT[ms][:, dc, tt * 128:(tt + 1) * 128], w_out[:, dc, h2 * 512:(h2 + 1) * 512],
                           dc == 0, dc == 7, [B_MT[ms], B_wC], [B_P[4 + h2]])
                R1, B_R1 = R1s[i % 2], B_R1s[i % 2]
                for h2 in range(2):
                    op("vector", lambda e: e.scalar_tensor_tensor(out=R1[:, h2 * 512:(h2 + 1) * 512],
                                                                  in0=xo[hs][:, h2 * 512:(h2 + 1) * 512],
                                                                  scalar=ALPHA, in1=P[4 + h2][:],
                                                                  op0=ALU.mult, op1=ALU.add),
                       reads=[B_xo[hs], B_P[4 + h2]], writes=[B_R1])
                ln_p1a(i, R1, B_R1, B_wC)

            def stage1b(i):
                h3 = i % 3
                ln_p1b(i, R1s[i % 2], B_R1s[i % 2], H1[h3], B_H1[h3])

            def stage1c(i):
                h3 = i % 3
                ln_p2(H1[h3], B_H1[h3], lng, lnb, B_wC)
                dma("sync", lambda e: e.dma_start(out=h1_d[i * 128:(i + 1) * 128, :], in_=H1[h3][:]),
                    reads=[B_H1[h3]], lane=B_H1[h3])

            def stage2a(i):
                hs = i % 3
                for hb in range(2):
                    ptr = P[6 + hb][:, :].rearrange("p (a t) -> p a t", a=4)
                    for a in range(4):
                        dc = hb * 4 + a
                        op("tensor", lambda e: e.transpose(out=ptr[:, a, :], in_=H1[hs][:, dc * 128:(dc + 1) * 128],
                                                           identity=identf[:]),
                           reads=[B_H1[hs], B_const], writes=[B_P[6 + hb]])
                    op("scalar", lambda e: e.activation(out=H1T[:, hb * 4:(hb + 1) * 4, :], in_=ptr, func=AF.Copy),
                       reads=[B_P[6 + hb]], writes=[B_H1T[hb]])
                for dc in range(8):
                    mm(P[2][:, 0:36], H1T[:, dc, :], w_r[:, dc, :], dc == 0, dc == 7,
                       [B_H1T[dc // 4], B_wC], [B_P[2]])

            def stage2b(i):
                op("vector", lambda e: e.tensor_tensor(out=LG[:, i, :], in0=P[2][:, 0:36], in1=brb[:], op=ALU.add),
                   reads=[B_P[2], B_wC], writes=[B_LG])

            gctr = [0]

            def gate_chunk(Q, m):
                s = 0
                pg = gctr[0] % 2
                gctr[0] += 1
                for c in range(8):
                    mm(P[pg][:], w_gates[:, c, m * 128:(m + 1) * 128], xbo[s][:, c, :], c == 0, c == 7,
                       [B_wG, B_xbo[s]], [B_P[pg]])
                op("scalar", lambda e: e.activation(out=Gt[:, m, :], in_=P[pg][:], func=AF.Sigmoid),
                   reads=[B_P[pg]], writes=[B_G[m]])

            def merge_dc(Q, dc):
                s = 0
                g, hh = dc // 2, dc % 2
                py_, pm_ = (2, 3) if dc % 2 == 0 else (6, 7)
                At, Bt, B_At, B_Bt = At2[dc % 2], Bt2[dc % 2], B_At2[dc % 2], B_Bt2[dc % 2]
                mm(P[py_][:], w_pm[:, g, hh * 128:(hh + 1) * 128], PLb[s][:, g, :], True, True,
                   [B_wC, B_PLb[s]], [B_P[py_]])
                for pr in range(4):
                    mm(P[pm_][:], w_mo[:, pr, dc * 128:(dc + 1) * 128], OT[:, pr, Q * 512:(Q + 1) * 512],
                       pr == 0, pr == 3, [B_wC, B_OT[pr]], [B_P[pm_]])
                op("vector", lambda e: e.scalar_tensor_tensor(out=At[:], in0=P[py_][:], scalar=psc[:, dc:dc + 1],
                                                              in1=Gt[:, dc, :], op0=ALU.mult, op1=ALU.mult),
                   reads=[B_P[py_], B_wC, B_G[dc]], writes=[B_At])
                op("vector", lambda e: e.tensor_tensor(out=Bt[:], in0=P[pm_][:], in1=Gt[:, 8 + dc, :], op=ALU.mult),
                   reads=[B_P[pm_], B_G[8 + dc]], writes=[B_Bt])
                op("vector", lambda e: e.tensor_tensor(out=MT[Q % 2][:, dc, :], in0=At[:], in1=Bt[:], op=ALU.add),
                   reads=[B_At, B_Bt], writes=[B_MT[Q % 2]])

            loadC_x(0)
            loadC_p(0)
            pending2 = []
            for m in range(16):
                gate_chunk(0, m)
            loadC_x(1)
            for Q in range(8):
                for dc in range(8):
                    merge_dc(Q, dc)
                    if Q + 1 < 8:
                        gate_chunk(Q + 1, dc)
                        gate_chunk(Q + 1, 8 + dc)
                if Q + 1 < 8:
                    loadC_p(Q + 1)
                if Q + 2 < 8:
                    loadC_x(Q + 2)
                for tt in range(4):
                    i = Q * 4 + tt
                    stage1a(i, Q % 2)
                    p2 = pending2.pop(0) if len(pending2) > 1 else None
                    if p2 is not None:
                        stage2a(p2)
                    if i >= 1:
                        stage1c(i - 1)
                    stage1b(i)
                    if p2 is not None:
                        stage2b(p2)
                    pending2.append(i)
            stage1c(NT - 1)
            while pending2:
                p2 = pending2.pop(0)
                stage2a(p2)
                stage2b(p2)
            T.barrier()

        with contextlib.ExitStack() as cC:
            P = [pst(cC, "PR%d" % i, [128, 512], F32) for i in range(4)]
            B_P = [T.buf("PR%d" % i) for i in range(4)]
            B_wC = T.buf("wC2")

            def rt(name, shape, dt=F32):
                return sbt(cC, name, shape, dt)
            B_r = T.buf("rtmp")

            def vop(fn, eng="vector"):
                return op(eng, fn, reads=[B_r, B_LG, B_const, B_wC], writes=[B_r])

            GL = LG[:, :, 0:4]
            gmax = rt("gmax", [128, NT])
            ohg = rt("ohg", [128, NT, 4])
            gd = rt("gd", [128, NT, 4])
            gsum = rt("gsum", [128, NT])
            gw = rt("gw", [128, NT])
            tmp4 = rt("tmp4", [128, NT, 4, 8])
            SEL = rt("SEL", [128, NT, 8])
            SEL2 = rt("SEL2", [128, NT, 8])
            m1 = rt("m1", [128, NT])
            m2 = rt("m2", [128, NT])
            oh1 = rt("oh1", [128, NT, 8])
            oh2 = rt("oh2", [128, NT, 8])
            rr = rt("rr", [128, NT])
            rinv = rt("rinv", [128, NT])
            E1 = rt("E1", [128, NT, 4, 8])
            E2 = rt("E2", [128, NT, 4, 8])
            EEb = rt("EEb", [128, NT, 32], BF16)
            lstr = rt("lstr", [128, 128], BF16)
            CUM = rt("CUM", [128, NT, 32])
            TOT = rt("TOT", [128, NT, 32])
            TP = rt("TP", [128, NT, 32])
            cnt = rt("cnt", [128, 32])
            b128i = rt("b128i", [128, NBLK], I32)
            b128 = rt("b128", [128, NBLK])
            pioi = rt("pioi", [128, 1], I32)
            pio = rt("pio", [128, 1])
            cmpk = rt("cmpk", [128, 32, 32])
            padded = rt("padded", [128, 32])
            sc0 = rt("sc0", [128, 32])
            sc1 = rt("sc1", [128, 32])
            pstart = rt("pstart", [128, 32])
            POS = rt("POS", [128, NT, 32])
            D1f = rt("D1f", [128, NT])
            D2f = rt("D2f", [128, NT])
            cmpb = rt("cmpb", [128, NBLK, 32])
            BE = rt("BE", [128, NBLK])

            dma("gpsimd", lambda e: e.dma_start(out=lstr[:], in_=lstrict_d[:, :]), writes=[B_r], lane=B_r)
            vop(lambda e: e.iota(b128i[:], pattern=[[BLKR, NBLK]], base=0, channel_multiplier=0), "gpsimd")
            vop(lambda e: e.iota(pioi[:], pattern=[[0, 1]], base=0, channel_multiplier=1), "gpsimd")
            vop(lambda e: e.tensor_copy(out=b128[:], in_=b128i[:]))
            vop(lambda e: e.tensor_copy(out=pio[:], in_=pioi[:]))

            def bc3(ap2, n):
                return ap2.unsqueeze(2).broadcast_to([128, NT, n])

            vop(lambda e: e.tensor_reduce(out=gmax[:], in_=GL, axis=AX.X, op=ALU.max))
            vop(lambda e: e.tensor_tensor(out=ohg[:], in0=GL, in1=bc3(gmax[:], 4), op=ALU.is_equal))
            vop(lambda e: e.tensor_tensor(out=gd[:], in0=GL, in1=bc3(gmax[:], 4), op=ALU.subtract))
            vop(lambda e: e.activation(out=gd[:], in_=gd[:], func=AF.Exp), "scalar")
            vop(lambda e: e.tensor_reduce(out=gsum[:], in_=gd[:], axis=AX.X, op=ALU.add))
            vop(lambda e: e.reciprocal(out=gw[:], in_=gsum[:]))
            EL = LG[:, :, 4:36].rearrange("p t (g j) -> p t g j", g=4)
            vop(lambda e: e.tensor_tensor(out=tmp4[:], in0=EL, in1=ohg[:].unsqueeze(3).broadcast_to([128, NT, 4, 8]),
                                          op=ALU.mult))
            vop(lambda e: e.tensor_reduce(out=SEL[:], in_=tmp4[:].rearrange("p t g j -> p t j g"), axis=AX.X, op=ALU.add))
            vop(lambda e: e.tensor_reduce(out=m1[:], in_=SEL[:], axis=AX.X, op=ALU.max))
            vop(lambda e: e.tensor_tensor(out=oh1[:], in0=SEL[:], in1=bc3(m1[:], 8), op=ALU.is_equal))
            vop(lambda e: e.scalar_tensor_tensor(out=SEL2[:], in0=oh1[:], scalar=-1.0e30, in1=SEL[:],
                                                 op0=ALU.mult, op1=ALU.add))
            vop(lambda e: e.tensor_reduce(out=m2[:], in_=SEL2[:], axis=AX.X, op=ALU.max))
            vop(lambda e: e.tensor_tensor(out=oh2[:], in0=SEL2[:], in1=bc3(m2[:], 8), op=ALU.is_equal))
            vop(lambda e: e.tensor_tensor(out=rr[:], in0=m2[:], in1=m1[:], op=ALU.subtract))
            vop(lambda e: e.activation(out=rr[:], in_=rr[:], func=AF.Exp), "scalar")
            hinR = [rt("hinR%d" % i, [128, D]) for i in range(4)]
            B_hinR = [T.buf("hinR%d" % i) for i in range(4)]
            hbA = rt("hbA", [128, NT, D], BF16)
            B_hbA = [T.buf("hbA%d" % i) for i in range(NT)]
            B_sc = [T.buf("sc%d" % i) for i in range(4)]
            for i in range(NT):
                s4 = i % 4
                dma("sync", lambda e: e.dma_start(out=hinR[s4][:], in_=h1_d[i * 128:(i + 1) * 128, :]),
                    writes=[B_hinR[s4]], lane=B_hinR[s4])
                op("scalar", lambda e: e.activation(out=hbA[:, i, :], in_=hinR[s4][:], func=AF.Copy),
                   reads=[B_hinR[s4]], writes=[B_hbA[i]])
            vop(lambda e: e.tensor_scalar(out=rinv[:], in0=rr[:], scalar1=1.0, scalar2=None, op0=ALU.add))
            vop(lambda e: e.reciprocal(out=rinv[:], in_=rinv[:]))
            vop(lambda e: e.tensor_tensor(out=G1[:], in0=gw[:], in1=rinv[:], op=ALU.mult))
            vop(lambda e: e.tensor_tensor(out=G2[:], in0=G1[:], in1=rr[:], op=ALU.mult))
            ohg4 = ohg[:].unsqueeze(3).broadcast_to([128, NT, 4, 8])
            vop(lambda e: e.tensor_tensor(out=E1[:], in0=ohg4, in1=oh1[:].unsqueeze(2).broadcast_to([128, NT, 4, 8]),
                                          op=ALU.mult))
            vop(lambda e: e.tensor_tensor(out=E2[:], in0=ohg4, in1=oh2[:].unsqueeze(2).broadcast_to([128, NT, 4, 8]),
                                          op=ALU.mult))
            E1v = E1[:].rearrange("p t g j -> p t (g j)")
            E2v = E2[:].rearrange("p t g j -> p t (g j)")
            vop(lambda e: e.tensor_tensor(out=EEb[:], in0=E1v, in1=E2v, op=ALU.add))
            for hb in range(2):
                rhs = EEb[:, hb * 16:(hb + 1) * 16, :].rearrange("p t e -> p (t e)")
                op("tensor", lambda e: e.matmul(P[hb][:], lhsT=lstr[:], rhs=rhs, start=True, stop=True),
                   reads=[B_r], writes=[B_P[hb]])
                op("tensor", lambda e: e.matmul(P[2 + hb][:], lhsT=onesb[:], rhs=rhs, start=True, stop=True),
                   reads=[B_r, B_const], writes=[B_P[2 + hb]])
            for hb in range(2):
                op("vector", lambda e: e.tensor_copy(out=CUM[:, hb * 16:(hb + 1) * 16, :].rearrange("p t e -> p (t e)"),
                                                     in_=P[hb][:]), reads=[B_P[hb], B_r], writes=[B_r])
                op("vector", lambda e: e.tensor_copy(out=TOT[:, hb * 16:(hb + 1) * 16, :].rearrange("p t e -> p (t e)"),
                                                     in_=P[2 + hb][:]), reads=[B_P[2 + hb], B_r], writes=[B_r])
            vop(lambda e: e.memset(TP[:, 0, :], 0.0))
            for i in range(1, NT):
                vop(lambda e: e.tensor_tensor(out=TP[:, i, :], in0=TP[:, i - 1, :], in1=TOT[:, i - 1, :], op=ALU.add))
            vop(lambda e: e.tensor_tensor(out=cnt[:], in0=TP[:, NT - 1, :], in1=TOT[:, NT - 1, :], op=ALU.add))
            vop(lambda e: e.tensor_tensor(out=cmpk[:], in0=cnt[:].unsqueeze(2).broadcast_to([128, 32, 32]),
                                          in1=b128[:, 0:32].unsqueeze(1).broadcast_to([128, 32, 32]), op=ALU.is_gt))
            vop(lambda e: e.tensor_reduce(out=padded[:], in_=cmpk[:], axis=AX.X, op=ALU.add))
            vop(lambda e: e.tensor_scalar(out=padded[:], in0=padded[:], scalar1=float(BLKR), scalar2=None, op0=ALU.mult))
            vop(lambda e: e.tensor_copy(out=sc0[:], in_=padded[:]))
            cur, nxt = sc0, sc1
            for sh in (1, 2, 4, 8, 16):
                vop(lambda e: e.tensor_copy(out=nxt[:, 0:sh], in_=cur[:, 0:sh]))
                vop(lambda e: e.tensor_tensor(out=nxt[:, sh:32], in0=cur[:, sh:32], in1=cur[:, 0:32 - sh], op=ALU.add))
                cur, nxt = nxt, cur
            pend_t = cur
            vop(lambda e: e.tensor_tensor(out=pstart[:], in0=pend_t[:], in1=padded[:], op=ALU.subtract))
            vop(lambda e: e.tensor_tensor(out=POS[:], in0=CUM[:], in1=TP[:], op=ALU.add))
            vop(lambda e: e.tensor_tensor(out=POS[:], in0=POS[:], in1=pstart[:].unsqueeze(1).broadcast_to([128, NT, 32]),
                                          op=ALU.add))
            vop(lambda e: e.tensor_tensor(out=CUM[:], in0=POS[:], in1=E1v, op=ALU.mult))
            vop(lambda e: e.tensor_reduce(out=D1f[:], in_=CUM[:], axis=AX.X, op=ALU.add))
            vop(lambda e: e.tensor_tensor(out=CUM[:], in0=POS[:], in1=E2v, op=ALU.mult))
            vop(lambda e: e.tensor_reduce(out=D2f[:], in_=CUM[:], axis=AX.X, op=ALU.add))
            op("vector", lambda e: e.tensor_copy(out=D1i[:], in_=D1f[:]), reads=[B_r], writes=[B_rt])
            op("vector", lambda e: e.tensor_copy(out=D2i[:], in_=D2f[:]), reads=[B_r], writes=[B_rt])
            vop(lambda e: e.tensor_tensor(out=cmpb[:], in0=pend_t[:].unsqueeze(1).broadcast_to([128, NBLK, 32]),
                                          in1=b128[:].unsqueeze(2).broadcast_to([128, NBLK, 32]), op=ALU.is_le))
            vop(lambda e: e.tensor_reduce(out=BE[:], in_=cmpb[:], axis=AX.X, op=ALU.add))
            vop(lambda e: e.tensor_scalar(out=BE[:], in0=BE[:], scalar1=128.0, scalar2=None, op0=ALU.mult))
            vop(lambda e: e.tensor_scalar(out=BE[:], in0=BE[:], scalar1=pio[:, 0:1], scalar2=None, op0=ALU.add))
            op("vector", lambda e: e.tensor_copy(out=IDXW[:], in_=BE[:]), reads=[B_r], writes=[B_rt])
            for i in range(NT):
                for Di in (D1i, D2i):
                    dma("gpsimd", lambda e: e.indirect_dma_start(
                        out=xs_d[:, :], out_offset=bass.IndirectOffsetOnAxis(ap=Di[:, i:i + 1], axis=0),
                        in_=hbA[:, i, :], in_offset=None), reads=[B_hbA[i], B_rt], lane=B_sc[i % 4])
            if DEBUG:
                dma("sync", lambda e: e.dma_start(out=d_lg[:, :, :], in_=LG[:]), reads=[B_LG], lane=B_LG)
                dma("sync", lambda e: e.dma_start(out=d_rt[:, 0, :], in_=D1f[:]), reads=[B_r], lane=B_r)
                dma("sync", lambda e: e.dma_start(out=d_rt[:, 1, :], in_=D2f[:]), reads=[B_r], lane=B_r)
                dma("sync", lambda e: e.dma_start(out=d_rt[:, 2, :], in_=G1[:]), reads=[B_r], lane=B_r)
                dma("sync", lambda e: e.dma_start(out=d_rt[:, 3, :], in_=G2[:]), reads=[B_r], lane=B_r)
                dma("sync", lambda e: e.dma_start(out=d_idx[:, :], in_=IDXW[:]), reads=[B_rt], lane=B_rt)
            T.barrier()
            if STOP == "C":
                return

        with contextlib.ExitStack() as cD:
            hin = [sbt(cD, "hin%d" % i, [128, D], F32) for i in range(4)]
            B_hin = [T.buf("hin%d" % i) for i in range(4)]
            with contextlib.ExitStack() as cD1:
                NW = 3
                WALL = [sbt(cD1, "WALL%d" % i, [128, 3 * 4096], BF16) for i in range(NW)]
                WG = [w_[:, 0:4096].rearrange("p (c n) -> p c n", c=8) for w_ in WALL]
                WU = [w_[:, 4096:8192].rearrange("p (c n) -> p c n", c=8) for w_ in WALL]
                WD = [w_[:, 8192:12288].rearrange("p (f n) -> p f n", f=4) for w_ in WALL]
                B_Wgu = [T.buf("W%d" % i) for i in range(NW)]
                B_Wd = B_Wgu
                NX = 4
                xsb = [sbt(cD1, "xsb%d" % i, [128, D], BF16) for i in range(NX)]
                B_xsb = [T.buf("xsb%d" % i) for i in range(NX)]
                XBT = [sbt(cD1, "XBT%d" % i, [128, 8, 128], BF16) for i in range(2)]
                B_XBT = [T.buf("XBT0"), T.buf("XBT1")]
                SG = [sbt(cD1, "SG%d" % i, [128, 512], F32) for i in range(2)]
                B_SG = [T.buf("SG0"), T.buf("SG1")]
                HT = [sbt(cD1, "HT%d" % i, [128, 4, 128], BF16) for i in range(2)]
                B_HT = [T.buf("HT0"), T.buf("HT1")]
                YB = [sbt(cD1, "YB%d" % i, [128, D], F32) for i in range(2)]
                B_YB = [T.buf("YB0"), T.buf("YB1")]
                PTB = pst(cD1, "PTB", [128, 8, 128], BF16)
                B_PTB = T.buf("PTB")
                HID = [sbt(cD1, "HID%d" % i, [128, 512], BF16) for i in range(2)]
                B_HID = [T.buf("HID0"), T.buf("HID1")]
                P = [pst(cD1, "PD%d" % i, [128, 512], F32) for i in range(6)]
                B_P = [T.buf("PD%d" % i) for i in range(6)]
                PTH = pst(cD1, "PTH", [128, 4, 128], BF16)
                B_PTH = T.buf("PTH")

                bc_reg = nc.gpsimd.to_reg(4095)

                def wload(b):
                    w = b % NW
                    dma("gpsimd", lambda e: e.indirect_dma_start(
                        out=WALL[w][:], out_offset=None, in_=w16_all[:, :],
                        in_offset=bass.IndirectOffsetOnAxis(ap=IDXW[:, b:b + 1], axis=0),
                        bounds_check=bc_reg, oob_is_err=False),
                        reads=[B_rt], writes=[B_Wgu[w]], lane=B_Wgu[w])

                def xload(b):
                    s = b % NX
                    dma("sync", lambda e: e.dma_start(out=xsb[s][:], in_=xs_d[b * 128:(b + 1) * 128, :]),
                        writes=[B_xsb[s]], lane=B_xsb[s])

                def stage_T(b):
                    s = b % 2
                    sx = b % NX
                    for c in range(8):
                        op("tensor", lambda e: e.transpose(out=PTB[:, c, :], in_=xsb[sx][:, c * 128:(c + 1) * 128],
                                                           identity=identb[:]),
                           reads=[B_xsb[sx], B_const], writes=[B_PTB])
                    op("vector", lambda e: e.tensor_copy(out=XBT[s][:, 0:4, :], in_=PTB[:, 0:4, :]),
                       reads=[B_PTB], writes=[B_XBT[s]])
                    op("scalar", lambda e: e.activation(out=XBT[s][:, 4:8, :], in_=PTB[:, 4:8, :], func=AF.Copy),
                       reads=[B_PTB], writes=[B_XBT[s]])

                def stage_GU(b):
                    s, w = b % 2, (b // 2) % NW
                    pg_, pu_ = 0 + 2 * s, 1 + 2 * s
                    for (pi, wt) in ((pg_, WG[w]), (pu_, WU[w])):
                        for c in range(8):
                            mm(P[pi][:], XBT[s][:, c, :], wt[:, c, :], c == 0, c == 7,
                               [B_Wgu[w], B_XBT[s]], [B_P[pi]])
                    op("scalar", lambda e: e.activation(out=SG[s][:], in_=P[pg_][:], func=AF.Silu),
                       reads=[B_P[pg_]], writes=[B_SG[s]])
                    op("vector", lambda e: e.tensor_tensor(out=HID[s][:], in0=P[pu_][:], in1=SG[s][:], op=ALU.mult),
                       reads=[B_P[pu_], B_SG[s]], writes=[B_HID[s]])

                def stage_HT(b):
                    s = b % 2
                    for f in range(4):
                        op("tensor", lambda e: e.transpose(out=PTH[:, f, :], in_=HID[s][:, f * 128:(f + 1) * 128],
                                                           identity=identb[:]),
                           reads=[B_HID[s], B_const], writes=[B_PTH])
                    op("vector", lambda e: e.tensor_copy(out=HT[s][:], in_=PTH[:]),
                       reads=[B_PTH], writes=[B_HT[s]])

                def stage_D(b):
                    s, w = b % 2, (b // 2) % NW
                    for h2 in range(2):
                        for f in range(4):
                            mm(P[4 + h2][:], HT[s][:, f, :], WD[w][:, f, h2 * 512:(h2 + 1) * 512], f == 0, f == 3,
                               [B_HT[s], B_Wd[w]], [B_P[4 + h2]])
                    op("scalar", lambda e: e.activation(out=YB[s][:, 0:512], in_=P[4][:], func=AF.Copy),
                       reads=[B_P[4]], writes=[B_YB[s]])
                    op("vector", lambda e: e.tensor_copy(out=YB[s][:, 512:1024], in_=P[5][:]),
                       reads=[B_P[5]], writes=[B_YB[s]])
                    dma("sync", lambda e: e.dma_start(out=ys_d[b * 128:(b + 1) * 128, :], in_=YB[s][:]),
                        reads=[B_YB[s]], lane=B_YB[s])

                NB_ = NBLK_RUN
                NTL = 2 * NB_
                wload(0)
                if NB_ > 1:
                    wload(1)
                xload(0)
                xload(1)
                xload(2)
                stage_T(0)
                stage_GU(0)
                if NTL > 1:
                    stage_T(1)
                for rt_ in range(NTL):
                    if rt_ % 2 == 0 and rt_ // 2 + 2 < NB_:
                        wload(rt_ // 2 + 2)
                    if rt_ + 3 < NTL:
                        xload(rt_ + 3)
                    if rt_ + 1 < NTL:
                        stage_GU(rt_ + 1)
                    stage_HT(rt_)
                    if rt_ + 2 < NTL:
                        stage_T(rt_ + 2)
                    stage_D(rt_)
                T.barrier()
                if STOP == "D1":
                    return

            with contextlib.ExitStack() as cD2:
                lng2 = sbt(cD2, "lng2", [128, D], F32)
                lnb2 = sbt(cD2, "lnb2", [128, D], F32)
                B_w2 = T.buf("w2")
                dma("sync", lambda e: e.dma_start(out=lng2[:], in_=ln2g_d.broadcast_to([128, D])), writes=[B_w2], lane=B_w2)
                dma("sync", lambda e: e.dma_start(out=lnb2[:], in_=ln2b_d.broadcast_to([128, D])), writes=[B_w2], lane=B_w2)
                Y1 = [sbt(cD2, "Y1_%d" % i, [128, D], F32) for i in range(3)]
                Y2 = [sbt(cD2, "Y2_%d" % i, [128, D], F32) for i in range(3)]
                B_Y1 = [T.buf("Y1%d" % i) for i in range(3)]
                B_Y2 = [T.buf("Y2%d" % i) for i in range(3)]
                R2s = [sbt(cD2, "R2_%d" % i, [128, D], F32) for i in range(2)]
                B_R2s = [T.buf("R2_0"), T.buf("R2_1")]
                OU = [sbt(cD2, "OU%d" % i, [128, D], F32) for i in range(2)]
                B_OU = [T.buf("OU0"), T.buf("OU1")]
                sts = [sbt(cD2, "st2_%d" % i, [128, 2, 6], F32) for i in range(2)]
                mvs = [sbt(cD2, "mv2_%d" % i, [128, 2], F32) for i in range(2)]
                rstds = [sbt(cD2, "rstd2_%d" % i, [128, 1], F32) for i in range(2)]
                nbs = [sbt(cD2, "nbias2_%d" % i, [128, 1], F32) for i in range(2)]
                vepss = [sbt(cD2, "veps2_%d" % i, [128, 1], F32) for i in range(2)]
                B_sts = [T.buf("st2_0"), T.buf("st2_1")]
                B_mvs = [T.buf("mv2_0"), T.buf("mv2_1")]
                B_rstds = [T.buf("rstd2_0"), T.buf("rstd2_1")]
                B_nbs = [T.buf("nb2_0"), T.buf("nb2_1")]
                B_vepss = [T.buf("veps2_0"), T.buf("veps2_1")]
                mhalf2 = sbt(cD2, "mhalf2", [128, 1], F32)
                B_mh2 = T.buf("mhalf2")
                op("gpsimd", lambda e: e.memset(mhalf2[:], -0.5), writes=[B_mh2])

                def loadD2(i):
                    s = i % 3
                    dma("sync", lambda e: e.dma_start(out=hin[s][:], in_=h1_d[i * 128:(i + 1) * 128, :]),
                        writes=[B_hin[s]], lane=B_hin[s])
                    for (Yt, By, Di) in ((Y1, B_Y1, D1i), (Y2, B_Y2, D2i)):
                        dma("gpsimd", lambda e: e.indirect_dma_start(
                            out=Yt[s][:], out_offset=None, in_=ys_d[:, :],
                            in_offset=bass.IndirectOffsetOnAxis(ap=Di[:, i:i + 1], axis=0)),
                            reads=[B_rt], writes=[By[s]], lane=By[s])

                def d2_part0(i):
                    s, s3 = i % 2, i % 3
                    op("scalar", lambda e: e.activation(out=R2s[s][:], in_=hin[s3][:], func=AF.Copy, scale=ALPHA),
                       reads=[B_hin[s3]], writes=[B_R2s[s]])

                def d2_part1(i):
                    s, s3 = i % 2, i % 3
                    R2, B_R2, st, mv, rstd, nbias2, veps2 = R2s[s], B_R2s[s], sts[s], mvs[s], rstds[s], nbs[s], vepss[s]
                    B_st, B_mv, B_rstd, B_nb2, B_veps2 = B_sts[s], B_mvs[s], B_rstds[s], B_nbs[s], B_vepss[s]
                    op("vector", lambda e: e.scalar_tensor_tensor(out=R2[:], in0=Y1[s3][:], scalar=G1[:, i:i + 1],
                                                                  in1=R2[:], op0=ALU.mult, op1=ALU.add),
                       reads=[B_Y1[s3], B_R2, B_rt], writes=[B_R2])
                    op("vector", lambda e: e.scalar_tensor_tensor(out=R2[:], in0=Y2[s3][:], scalar=G2[:, i:i + 1],
                                                                  in1=R2[:], op0=ALU.mult, op1=ALU.add),
                       reads=[B_Y2[s3], B_R2, B_rt], writes=[B_R2])
                    for c in range(2):
                        op("vector", lambda e: e.bn_stats(out=st[:, c, :], in_=R2[:, c * 512:(c + 1) * 512]),
                           reads=[B_R2], writes=[B_st])
                    op("vector", lambda e: e.bn_aggr(out=mv[:, :], in_=st[:, :, :].rearrange("p a b -> p (a b)")),
                       reads=[B_st], writes=[B_mv])
                    op("gpsimd", lambda e: e.tensor_scalar(out=veps2[:], in0=mv[:, 1:2], scalar1=EPS, scalar2=None,
                                                           op0=ALU.add), reads=[B_mv], writes=[B_veps2])
                    op("gpsimd", lambda e: e.tensor_tensor(out=rstd[:], in0=veps2[:], in1=mhalf2[:], op=ALU.pow),
                       reads=[B_veps2, B_mh2], writes=[B_rstd])

                def d2_part1b(i):
                    s = i % 2
                    R2, B_R2, mv, rstd, nbias2 = R2s[s], B_R2s[s], mvs[s], rstds[s], nbs[s]
                    B_mv, B_rstd, B_nb2 = B_mvs[s], B_rstds[s], B_nbs[s]
                    op("vector", lambda e: e.scalar_tensor_tensor(out=nbias2[:], in0=mv[:, 0:1], scalar=-1.0, in1=rstd[:],
                                                                  op0=ALU.mult, op1=ALU.mult),
                       reads=[B_mv, B_rstd], writes=[B_nb2])
                    op("scalar", lambda e: e.activation(out=OU[s][:], in_=R2[:], func=AF.Identity, scale=rstd[:, 0:1],
                                                        bias=nbias2[:, 0:1]),
                       reads=[B_R2, B_nb2, B_rstd], writes=[B_OU[s]])

                def d2_part2(i):
                    s = i % 2
                    op("vector", lambda e: e.tensor_tensor(out=OU[s][:], in0=OU[s][:], in1=lng2[:], op=ALU.mult),
                       reads=[B_OU[s], B_w2], writes=[B_OU[s]])
                    op("vector", lambda e: e.tensor_tensor(out=OU[s][:], in0=OU[s][:], in1=lnb2[:], op=ALU.add),
                       reads=[B_OU[s], B_w2], writes=[B_OU[s]])
                    dma("sync", lambda e: e.dma_start(out=out_d[i * 128:(i + 1) * 128, :], in_=OU[s][:]),
                        reads=[B_OU[s]], lane=B_OU[s])

                loadD2(0)
                loadD2(1)
                loadD2(2)
                d2_part0(0)
                d2_part1(0)
                d2_part1b(0)
                d2_part0(1)
                for i in range(NT):
                    if i + 3 < NT:
                        loadD2(i + 3)
                    if i + 2 < NT:
                        d2_part0(i + 2)
                    if i + 1 < NT:
                        d2_part1(i + 1)
                    d2_part2(i)
                    if i + 1 < NT:
                        d2_part1b(i + 1)
                T.barrier()

    with contextlib.ExitStack() as es:
        _body(es)
    return nc


def _kc(w, nk):
    n = w.shape[1]
    return np.ascontiguousarray(w.reshape(nk, 128, n).transpose(1, 0, 2))


def _const_tables(hf):
    inv = 1.0 / (10000.0 ** (np.arange(0, 32, 2, dtype=np.float64) / 32.0))
    pos_all = np.arange(S, dtype=np.float64)
    ang = pos_all[:, None] * inv[None, :]
    cos_a, sin_a = np.cos(ang), np.sin(ang)
    sign = np.concatenate([-np.ones(16), np.ones(16)])
    cos32 = np.concatenate([cos_a, cos_a], axis=1).T
    sin32 = (np.concatenate([sin_a, sin_a], axis=1) * sign[None, :]).T
    cosk = np.concatenate([cos32, cos32], axis=0).astype(np.float32)
    sink = np.concatenate([sin32, sin32], axis=0).astype(np.float32)
    loc = np.arange(NOWN)
    own_pos = (loc // 32) * 64 + hf * 32 + (loc % 32)
    tblq = np.empty((128, NOWN), np.float64)
    tblq[0:32] = cos32[:, own_pos] * SCALE
    tblq[32:64] = sin32[:, own_pos] * SCALE
    tblq[64:128] = SCALE
    wins = (2, 4, 8, 16)
    a_main = np.zeros((128, 4, 64)); a_halo = np.zeros((128, 4, 64)); a_first = np.zeros((128, 4, 64))
    for g, w in enumerate(wins):
        for j in range(64):
            t = 64 * (j // 32) + 32 * hf + (j % 32)
            for tp in range(t - w + 1, t + 1):
                if tp >= 0:
                    a_main[tp, g, j] += 1.0 / w
                    a_first[tp, g, j] += 1.0 / min(t + 1, w)
                else:
                    a_halo[128 + tp, g, j] += 1.0 / w
            a_main[t, g, j] -= 1.0
            a_first[t, g, j] -= 1.0
    return (cosk, sink, tblq.astype(np.float32), a_main.astype(np.float32), a_halo.astype(np.float32),
            a_first.astype(np.float32), own_pos)


def _prep_shared(w_in, pool_mix_w, pool_scale, q_norm_g, w_uq, kv_norm_g, w_ukv, w_mla_o, w_out,
                 ln1_g, ln1_b, w_router_group, b_router_group, w_router_expert, b_router_expert,
                 w_gate, w_up, w_down, ln2_g, ln2_b):
    f = lambda a: np.asarray(a, dtype=np.float32)
    w_in = f(w_in)[0]
    sh = {}
    sh["w_pool"] = _kc(w_in[:, 0:512], 8)
    sh["w_cq"] = _kc(w_in[:, 512:896], 8)
    sh["w_ckv"] = _kc(w_in[:, 896:1152], 8)
    kpe = w_in[:, 1152:1184]
    kpesw = np.concatenate([kpe[:, 16:32], kpe[:, 0:16]], axis=1)
    sh["w_kpe2"] = _kc(np.concatenate([kpe, kpe], axis=1), 8)
    sh["w_kpesw2"] = _kc(np.concatenate([kpesw, kpesw], axis=1), 8)
    sh["w_gates"] = _kc(w_in[:, 1184:3232], 8)
    uq = f(w_uq)[0].reshape(384, 8, 96)
    nope, pe = uq[:, :, 0:64], uq[:, :, 64:96]
    pesw = np.concatenate([pe[:, :, 16:32], pe[:, :, 0:16]], axis=2)
    uq_l = np.concatenate([pe, pesw, nope], axis=2)
    sh["w_uq_l"] = np.ascontiguousarray(uq_l.reshape(3, 128, 8, 128).transpose(1, 0, 2, 3))
    sh["qg"] = np.ascontiguousarray(f(q_norm_g)[0].reshape(3, 128).T)
    ukv = f(w_ukv)[0].reshape(256, 8, 128)
    sh["w_uk_l"] = np.ascontiguousarray(ukv[:, :, 0:64].reshape(2, 128, 8, 64).transpose(1, 0, 2, 3))
    sh["w_uv_l"] = np.ascontiguousarray(ukv[:, :, 64:128].reshape(2, 128, 8, 64).transpose(1, 0, 2, 3))
    sh["kvg"] = np.ascontiguousarray(f(kv_norm_g)[0].reshape(2, 128).T)
    sh["w_mo_l"] = _kc(f(w_mla_o)[0], 4)
    sh["w_out_l"] = _kc(f(w_out)[0], 8)
    sh["w_pm_l"] = np.ascontiguousarray(f(pool_mix_w)[0].transpose(1, 0, 2))
    sh["psc"] = np.ascontiguousarray(f(pool_scale)[0].reshape(8, 128).T)
    sh["ln1_g"] = f(ln1_g).reshape(1, D)
    sh["ln1_b"] = f(ln1_b).reshape(1, D)
    sh["ln2_g"] = f(ln2_g).reshape(1, D)
    sh["ln2_b"] = f(ln2_b).reshape(1, D)
    sh["w_r"] = _kc(np.concatenate([f(w_router_group)[0], f(w_router_expert)[0]], axis=1), 8)
    sh["b_r"] = np.concatenate([f(b_router_group)[0], f(b_router_expert)[0]]).reshape(1, 36)
    wg = f(w_gate)[0]
    wu = f(w_up)[0]
    wd = f(w_down)[0]
    sh["wg_l"] = np.ascontiguousarray(wg.reshape(32, 8, 128, 512).transpose(0, 2, 1, 3)).reshape(32 * 128, 4096)
    sh["wu_l"] = np.ascontiguousarray(wu.reshape(32, 8, 128, 512).transpose(0, 2, 1, 3)).reshape(32 * 128, 4096)
    sh["wd_l"] = np.ascontiguousarray(wd.reshape(32, 4, 128, 1024).transpose(0, 2, 1, 3)).reshape(32 * 128, 4096)
    sh["ident"] = np.eye(128, dtype=np.float32)
    sh["lstrict"] = np.triu(np.ones((128, 128), np.float32), k=1)
    return sh


def make_in_maps(inputs):
    x = np.asarray(inputs["x"], dtype=np.float32)
    sh = _prep_shared(**{k: v for k, v in inputs.items() if k != "x"})
    in_maps, own_positions = [], []
    consts = [_const_tables(hf) for hf in range(2)]
    for core in range(8):
        b, hf = core // 2, core % 2
        cosk, sink, tblq, a_main, a_halo, a_first, own_pos = consts[hf]
        xb = x[b]
        m = dict(sh)
        m["xT_all"] = np.ascontiguousarray(xb.T.reshape(8, 128, S).transpose(1, 0, 2))
        xo = xb[own_pos]
        m["x_own"] = np.ascontiguousarray(xo)
        m["xT_own"] = np.ascontiguousarray(xo.T.reshape(8, 128, NOWN).transpose(1, 0, 2))
        m["cosk"], m["sink"], m["tblq"] = cosk, sink, tblq
        m["a_main"], m["a_halo"], m["a_first"] = a_main, a_halo, a_first
        in_maps.append(m)
        own_positions.append(own_pos)
    return in_maps, own_positions


_NC_CACHE = {}


def kernel(**inputs):
    in_maps, own_positions = make_in_maps(inputs)
    if "nc" not in _NC_CACHE:
        _NC_CACHE["nc"] = build_program()
    nc = _NC_CACHE["nc"]
    res = run_bass_kernel_spmd(nc, in_maps, core_ids=list(range(8)))
    out = np.empty((B, S, D), np.float32)
    for core in range(8):
        b = core // 2
        out[b, own_positions[core], :] = np.asarray(res.results[core]["out"], dtype=np.float32)
    if DEBUG:
        kernel.last_results = res.results
    return out
```

```python
import contextlib
import math
import numpy as np
import ml_dtypes
import concourse.bass as bass
import concourse.mybir as mybir
from concourse.alu_op_type import AluOpType as ALU
from concourse.bass_utils import run_bass_kernel_spmd

F32 = mybir.dt.float32
BF16 = mybir.dt.bfloat16
I32 = mybir.dt.int32
AF = mybir.ActivationFunctionType
AX = mybir.AxisListType

D = 1024
S = 8192
B = 4
NOWN = 4096
NT = 32
BLKR = 256
NBLK = 64
R = NBLK * BLKR
EPS = 1e-5
ALPHA = 2.0 ** 0.25
SCALE = 96.0 ** -0.5
DEBUG = False
STOP = ""
NHEADS_RUN = 8
NBLK_RUN = NBLK


class _Stop(Exception):
    pass


class Buf:
    __slots__ = ("name", "w", "r", "dsem", "dtot")

    def __init__(self, name):
        self.name = name
        self.w = None
        self.r = {}
        self.dsem = None
        self.dtot = 0


class Eng:
    def __init__(self, name, eng, sem):
        self.name, self.eng, self.sem, self.cnt, self.seen = name, eng, sem, 0, {}


class TK:
    def __init__(self, nc, es):
        self.nc, self.es = nc, es
        self.E = {}
        for n in ("tensor", "vector", "scalar", "gpsimd", "sync"):
            self.E[n] = Eng(n, getattr(nc, n), es.enter_context(nc.semaphore("es_" + n)))
        self.lanes = []
        self.bufs = []
        self.bar = es.enter_context(nc.semaphore("barrier"))
        self.barcnt = 0
        self.nb = 0

    def buf(self, name=None):
        self.nb += 1
        b = Buf(name or ("b%d" % self.nb))
        self.bufs.append(b)
        return b

    def _wait(self, e, sem, val):
        k = id(sem)
        if e.seen.get(k, 0) >= val:
            return
        e.eng.wait_ge(sem, val)
        e.seen[k] = val

    def _need(self, e, rec, kind):
        sem, val, owner = rec
        if owner is e and (e.name == "tensor" or kind == "war"):
            return
        self._wait(e, sem, val)

    def _deps(self, e, reads, writes):
        for b in reads:
            if b.w is not None:
                self._need(e, b.w, "raw")
        for b in writes:
            if b.w is not None:
                self._need(e, b.w, "waw")
            for rec in b.r.values():
                self._need(e, rec, "war")

    def op(self, en, fn, reads=(), writes=()):
        e = self.E[en]
        self._deps(e, reads, writes)
        ins = fn(e.eng)
        e.cnt += 1
        ins.then_inc(e.sem, 1)
        rec = (e.sem, e.cnt, e)
        for b in reads:
            b.r[id(e.sem)] = rec
        for b in writes:
            b.w = rec
            b.r = {}
        return ins

    def _lane(self, lane):
        if lane.dsem is None:
            lane.dsem = self.es.enter_context(self.nc.semaphore("ds%d" % len(self.lanes)))
            self.lanes.append(lane)

    def dma(self, qn, fn, reads=(), writes=(), lane=None):
        e = self.E[qn]
        self._deps(e, reads, writes)
        self._lane(lane)
        ins = fn(e.eng)
        lane.dtot += 16
        ins.then_inc(lane.dsem, 16)
        rec = (lane.dsem, lane.dtot, None)
        for b in reads:
            b.r[id(lane.dsem)] = rec
        for b in writes:
            b.w = rec
            b.r = {}
        return ins

    def barrier(self):
        sy = self.E["sync"]
        for e in self.E.values():
            if e is not sy and e.cnt > 0:
                self._wait(sy, e.sem, e.cnt)
        for l in self.lanes:
            if l.dtot > 0:
                self._wait(sy, l.dsem, l.dtot)
        self.barcnt += 1
        sy.eng.sem_inc(self.bar, 1)
        for e in self.E.values():
            if e is not sy:
                e.eng.wait_ge(self.bar, self.barcnt)
        for b in self.bufs:
            b.w = None
            b.r = {}
        for e in self.E.values():
            for e2 in self.E.values():
                e.seen[id(e2.sem)] = e2.cnt
            for l in self.lanes:
                e.seen[id(l.dsem)] = l.dtot


def build_program():
    nc = bass.Bass("TRN2", target_bir_lowering=False)

    def din(name, shape, dt=F32):
        return nc.dram_tensor(name, list(shape), dt, kind="ExternalInput").ap()

    def dscr(name, shape, dt, dbg=False):
        kind = "ExternalOutput" if (dbg and DEBUG) else "Internal"
        return nc.dram_tensor(name, list(shape), dt, kind=kind).ap()

    xT_all = din("xT_all", [128, 8, S])
    xT_own = din("xT_own", [128, 8, NOWN])
    x_own = din("x_own", [NOWN, D])
    w_pool_d = din("w_pool", [128, 8, 512])
    w_cq_d = din("w_cq", [128, 8, 384])
    w_ckv_d = din("w_ckv", [128, 8, 256])
    w_kpe_d = din("w_kpe2", [128, 8, 64])
    w_kpesw_d = din("w_kpesw2", [128, 8, 64])
    w_gates_d = din("w_gates", [128, 8, 2048])
    w_uq_d = din("w_uq_l", [128, 3, 8, 128])
    qg_d = din("qg", [128, 3])
    w_uk_d = din("w_uk_l", [128, 2, 8, 64])
    w_uv_d = din("w_uv_l", [128, 2, 8, 64])
    kvg_d = din("kvg", [128, 2])
    w_mo_d = din("w_mo_l", [128, 4, 1024])
    w_out_d = din("w_out_l", [128, 8, 1024])
    w_pm_d = din("w_pm_l", [128, 4, 256])
    psc_d = din("psc", [128, 8])
    ln1g_d = din("ln1_g", [1, D])
    ln1b_d = din("ln1_b", [1, D])
    ln2g_d = din("ln2_g", [1, D])
    ln2b_d = din("ln2_b", [1, D])
    w_r_d = din("w_r", [128, 8, 36])
    b_r_d = din("b_r", [1, 36])
    wg_d = din("wg_l", [32 * 128, 4096])
    wu_d = din("wu_l", [32 * 128, 4096])
    wd_d = din("wd_l", [32 * 128, 4096])
    tblq_d = din("tblq", [128, NOWN])
    cosk_d = din("cosk", [64, S])
    sink_d = din("sink", [64, S])
    amain_d = din("a_main", [128, 4, 64])
    ahalo_d = din("a_halo", [128, 4, 64])
    afirst_d = din("a_first", [128, 4, 64])
    ident_d = din("ident", [128, 128])
    lstrict_d = din("lstrict", [128, 128])

    out_d = nc.dram_tensor("out", [NOWN, D], F32, kind="ExternalOutput").ap()
    poolT_d = dscr("poolT_s", [128, 4, NOWN], BF16)
    h1_d = dscr("h1_s", [NOWN, D], F32, dbg=True)
    xs_d = dscr("xs_s", [R, D], BF16)
    ys_d = dscr("ys_s", [R, D], F32)
    w16_all = dscr("w16_all", [32 * 128, 3 * 4096], BF16)
    wC16 = {"gates": dscr("wc_gates", [128, 8, 2048], BF16), "mo": dscr("wc_mo", [128, 4, 1024], BF16),
            "out": dscr("wc_out", [128, 8, 1024], BF16), "pm": dscr("wc_pm", [128, 4, 256], BF16)}
    if DEBUG:
        d_ckvn = dscr("d_ckvn", [128, 2, S], BF16, dbg=True)
        d_cqn = dscr("d_cqn", [128, 3, NOWN], BF16, dbg=True)
        d_kt = dscr("d_kt", [128, S], BF16, dbg=True)
        d_ot = dscr("d_ot", [128, 4, NOWN], BF16, dbg=True)
        d_lg = dscr("d_lg", [128, NT, 36], F32, dbg=True)
        d_rt = dscr("d_rt", [128, 4, NT], F32, dbg=True)
        d_idx = dscr("d_idx", [128, NBLK], I32, dbg=True)

    TREF = []

    def _body(es):
        T = TK(nc, es)
        TREF.append(T)
        op, dma = T.op, T.dma

        def sbt(ctx, name, shape, dt):
            return ctx.enter_context(nc.sbuf_tensor("s_" + name, list(shape), dt))

        def pst(ctx, name, shape, dt):
            return ctx.enter_context(nc.psum_tensor("p_" + name, list(shape), dt))

        def mm(out, lhsT, rhs, start, stop, reads, writes):
            return op("tensor", lambda e: e.matmul(out, lhsT=lhsT, rhs=rhs, start=start, stop=stop),
                      reads=reads, writes=writes)

        OT = sbt(es, "OT", [128, 4, NOWN], BF16)
        B_OT = [T.buf("OT%d" % i) for i in range(4)]
        D1i = sbt(es, "D1i", [128, NT], I32)
        D2i = sbt(es, "D2i", [128, NT], I32)
        G1 = sbt(es, "G1", [128, NT], F32)
        G2 = sbt(es, "G2", [128, NT], F32)
        IDXW = sbt(es, "IDXW", [128, NBLK], I32)
        B_rt = T.buf("route")
        LG = sbt(es, "LG", [128, NT, 36], F32)
        B_LG = T.buf("LG")
        identf = sbt(es, "identf", [128, 128], F32)
        identb = sbt(es, "identb", [128, 128], BF16)
        onesb = sbt(es, "onesb", [128, 128], BF16)
        B_const = T.buf("const")
        dma("sync", lambda e: e.dma_start(out=identf[:], in_=ident_d[:, :]), writes=[B_const], lane=B_const)
        dma("gpsimd", lambda e: e.dma_start(out=identb[:], in_=ident_d[:, :]), writes=[B_const], lane=B_const)
        op("vector", lambda e: e.memset(onesb[:], 1.0), writes=[B_const])

        with contextlib.ExitStack() as cAB:
            CKVN = sbt(cAB, "CKVN", [128, 2, S], BF16)
            CQN = sbt(cAB, "CQN", [128, 3, NOWN], BF16)
            KT = [sbt(cAB, "KT%d" % i, [128, S], BF16) for i in range(2)]
            B_ckvn, B_cqn = T.buf("ckvn"), T.buf("cqn")
            B_KTlo = [T.buf("KTlo0"), T.buf("KTlo1")]
            B_KThi = [T.buf("KThi0"), T.buf("KThi1")]

            with contextlib.ExitStack() as cA:
                w_ckv = sbt(cA, "w_ckv", [128, 8, 256], BF16)
                w_kpe = sbt(cA, "w_kpe", [128, 8, 64], BF16)
                w_kpesw = sbt(cA, "w_kpesw", [128, 8, 64], BF16)
                w_pool = sbt(cA, "w_pool", [128, 8, 512], BF16)
                w_cq = sbt(cA, "w_cq", [128, 8, 384], BF16)
                a_main = sbt(cA, "a_main", [128, 4, 64], BF16)
                a_halo = sbt(cA, "a_halo", [128, 4, 64], BF16)
                a_first = sbt(cA, "a_first", [128, 4, 64], BF16)
                BW = {}
                for nm_, t_, d_ in (("ckv", w_ckv, w_ckv_d), ("kpe", w_kpe, w_kpe_d), ("kpesw", w_kpesw, w_kpesw_d),
                                    ("pool", w_pool, w_pool_d), ("am", a_main, amain_d), ("ah", a_halo, ahalo_d),
                                    ("af", a_first, afirst_d), ("cq", w_cq, w_cq_d)):
                    BW[nm_] = T.buf("wA_" + nm_)
                    dma("gpsimd", lambda e, t_=t_, d_=d_: e.dma_start(out=t_[:], in_=d_[:, :, :]),
                        writes=[BW[nm_]], lane=BW[nm_])
                xb = [sbt(cA, "xb%d" % i, [128, 8, 512], BF16) for i in range(2)]
                B_xb = [T.buf("xb0"), T.buf("xb1")]
                cosb = [sbt(cA, "cosb%d" % i, [64, 512], F32) for i in range(2)]
                sinb = [sbt(cA, "sinb%d" % i, [64, 512], F32) for i in range(2)]
                B_cs = [T.buf("cs0"), T.buf("cs1")]
                sq = sbt(cA, "sq", [128, 3, 512], BF16)
                B_sq = [T.buf("sq0"), T.buf("sq1"), T.buf("sq2")]
                rs = sbt(cA, "rs", [128, 512], F32)
                B_rs = T.buf("rs")
                t1 = sbt(cA, "t1", [64, 512], F32)
                t2 = sbt(cA, "t2", [64, 512], F32)
                B_t1, B_t2 = T.buf("t1"), T.buf("t2")
                UT = [sbt(cA, "UT%d" % i, [128, 512], BF16) for i in range(6)]
                B_UT = [T.buf("UT%d" % i) for i in range(6)]
                PL = [sbt(cA, "PL%d" % i, [128, 4, 256], BF16) for i in range(2)]
                B_PL = [T.buf("PL0"), T.buf("PL1")]
                P = [pst(cA, "PA%d" % i, [128, 512], F32) for i in range(8)]
                B_P = [T.buf("PA%d" % i) for i in range(8)]

                def load_all(tb):
                    s = tb % 2
                    dma("gpsimd", lambda e: e.dma_start(out=xb[s][:], in_=xT_all[:, :, tb * 512:(tb + 1) * 512]),
                        writes=[B_xb[s]], lane=B_xb[s])
                    dma("sync", lambda e: e.dma_start(out=cosb[s][:], in_=cosk_d[:, tb * 512:(tb + 1) * 512]),
                        writes=[B_cs[s]], lane=B_cs[s])
                    dma("sync", lambda e: e.dma_start(out=sinb[s][:], in_=sink_d[:, tb * 512:(tb + 1) * 512]),
                        writes=[B_cs[s]], lane=B_cs[s])

                def norm_block(nm, wt, xs_, Bx, dest, B_dest, col0, nfeat):
                    Bw_ = BW["ckv"] if nm == 2 else BW["cq"]
                    for m in range(nm):
                        for c in range(8):
                            mm(P[m][:], wt[:, c, m * 128:(m + 1) * 128], xs_[:, c, :], c == 0, c == 7,
                               [Bw_, Bx], [B_P[m]])
                    for m in range(nm):
                        op("scalar", lambda e: e.activation(out=sq[:, m, :], in_=P[m][:], func=AF.Square),
                           reads=[B_P[m]], writes=[B_sq[m]])
                    return

                def norm_finish(nm, dest, B_dest, col0, nfeat):
                    for m in range(nm):
                        mm(P[3][:], onesb[:], sq[:, m, :], m == 0, m == nm - 1, [B_const, B_sq[m]], [B_P[3]])
                    op("scalar", lambda e: e.activation(out=rs[:], in_=P[3][:], func=AF.Sqrt,
                                                         scale=1.0 / nfeat, bias=EPS),
                       reads=[B_P[3]], writes=[B_rs])
                    op("vector", lambda e: e.reciprocal(out=rs[:], in_=rs[:]), reads=[B_rs], writes=[B_rs])
                    for m in range(nm):
                        op("vector", lambda e: e.tensor_tensor(out=dest[:, m, col0:col0 + 512], in0=P[m][:],
                                                               in1=rs[:], op=ALU.mult),
                           reads=[B_P[m], B_rs], writes=[B_dest])

                load_all(0)
                for tb in range(16):
                    s = tb % 2
                    if tb + 1 < 16:
                        load_all(tb + 1)
                    xs_ = xb[s]
                    c0 = tb * 512
                    norm_block(2, w_ckv, xs_, B_xb[s], CKVN, B_ckvn, c0, 256)
                    for (pi, wt, bn_) in ((4, w_kpe, "kpe"), (5, w_kpesw, "kpesw")):
                        for c in range(8):
                            mm(P[pi][0:64, :], wt[:, c, :], xs_[:, c, :], c == 0, c == 7, [BW[bn_], B_xb[s]], [B_P[pi]])
                    norm_finish(2, CKVN, B_ckvn, c0, 256.0)
                    op("vector", lambda e: e.tensor_tensor(out=t1[:], in0=P[4][0:64, :], in1=cosb[s][:], op=ALU.mult),
                       reads=[B_P[4], B_cs[s]], writes=[B_t1])
                    op("vector", lambda e: e.tensor_tensor(out=t2[:], in0=P[5][0:64, :], in1=sinb[s][:], op=ALU.mult),
                       reads=[B_P[5], B_cs[s]], writes=[B_t2])
                    op("gpsimd", lambda e: e.tensor_tensor(out=KT[0][0:64, c0:c0 + 512], in0=t1[:], in1=t2[:], op=ALU.add),
                       reads=[B_t1, B_t2], writes=[B_KTlo[0]])
                    op("gpsimd", lambda e: e.tensor_tensor(out=KT[1][0:64, c0:c0 + 512], in0=t1[:], in1=t2[:], op=ALU.add),
                       reads=[B_t1, B_t2], writes=[B_KTlo[1]])
                    for tt in range(4):
                        G = tb * 4 + tt
                        pu = 6 + (tt % 2)
                        for c in range(8):
                            mm(P[pu][:], xs_[:, c, tt * 128:(tt + 1) * 128], w_pool[:, c, :], c == 0, c == 7,
                               [BW["pool"], B_xb[s]], [B_P[pu]])
                        op("scalar", lambda e: e.activation(out=UT[G % 6][:], in_=P[pu][:], func=AF.Copy),
                           reads=[B_P[pu]], writes=[B_UT[G % 6]])
                    for tt in range(4):
                        G = tb * 4 + tt
                        pp = P[2][:, 0:256].rearrange("p (g j) -> p g j", g=4)
                        for g in range(4):
                            am = a_first if G == 0 else a_main
                            mm(pp[:, g, :], UT[G % 6][:, g * 128:(g + 1) * 128], am[:, g, :], True, G == 0,
                               [B_UT[G % 6], BW["af"], BW["am"]], [B_P[2]])
                            if G > 0:
                                mm(pp[:, g, :], UT[(G - 1) % 6][64:128, g * 128:(g + 1) * 128], a_halo[64:128, g, :],
                                   False, True, [B_UT[(G - 1) % 6], BW["ah"]], [B_P[2]])
                        op("vector", lambda e: e.tensor_copy(out=PL[s][:, :, tt * 64:(tt + 1) * 64], in_=pp),
                           reads=[B_P[2]], writes=[B_PL[s]])
                    dma("sync", lambda e: e.dma_start(out=poolT_d[:, :, tb * 256:(tb + 1) * 256], in_=PL[s][:]),
                        reads=[B_PL[s]], lane=B_PL[s])

                def load_own(qb):
                    s = qb % 2
                    dma("gpsimd", lambda e: e.dma_start(out=xb[s][:], in_=xT_own[:, :, qb * 512:(qb + 1) * 512]),
                        writes=[B_xb[s]], lane=B_xb[s])
                load_own(0)
                for qb in range(8):
                    s = qb % 2
                    if qb + 1 < 8:
                        load_own(qb + 1)
                    norm_block(3, w_cq, xb[s], B_xb[s], CQN, B_cqn, qb * 512, 384)
                    norm_finish(3, CQN, B_cqn, qb * 512, 384.0)
                if DEBUG:
                    dma("sync", lambda e: e.dma_start(out=d_ckvn[:, :, :], in_=CKVN[:]), reads=[B_ckvn], lane=B_ckvn)
                    dma("sync", lambda e: e.dma_start(out=d_cqn[:, :, :], in_=CQN[:]), reads=[B_cqn], lane=B_cqn)
                T.barrier()
                if STOP == "A":
                    return

            with contextlib.ExitStack() as cB:
                w_uq = sbt(cB, "w_uq", [128, 3, 8, 128], BF16)
                w_uk = sbt(cB, "w_uk", [128, 2, 8, 64], BF16)
                w_uv = sbt(cB, "w_uv", [128, 2, 8, 64], BF16)
                qg = sbt(cB, "qg", [128, 3], F32)
                kvg = sbt(cB, "kvg", [128, 2], F32)
                B_wB, B_st = T.buf("wB"), T.buf("stB")
                cSt = contextlib.ExitStack()
                stage = sbt(cSt, "stageB", [128, 3, 8, 128], F32)
                dma("sync", lambda e: e.dma_start(out=qg[:], in_=qg_d[:, :]), writes=[B_wB], lane=B_wB)
                dma("sync", lambda e: e.dma_start(out=kvg[:], in_=kvg_d[:, :]), writes=[B_wB], lane=B_wB)
                dma("sync", lambda e: e.dma_start(out=stage[:], in_=w_uq_d[:, :, :, :]), writes=[B_st], lane=B_st)
                for kc in range(3):
                    op("vector", lambda e: e.tensor_scalar_mul(out=w_uq[:, kc, :, :], in0=stage[:, kc, :, :],
                                                               scalar1=qg[:, kc:kc + 1]),
                       reads=[B_st, B_wB], writes=[B_wB])
                for (wt, wd_) in ((w_uk, w_uk_d), (w_uv, w_uv_d)):
                    stv = stage[:, 0:2, :, 0:64]
                    dma("sync", lambda e: e.dma_start(out=stv, in_=wd_[:, :, :, :]), writes=[B_st], lane=B_st)
                    for m in range(2):
                        op("vector", lambda e: e.tensor_scalar_mul(out=wt[:, m, :, :], in0=stage[:, m, :, 0:64],
                                                                   scalar1=kvg[:, m:m + 1]),
                           reads=[B_st, B_wB], writes=[B_wB])
                T.barrier()
                cSt.close()
                B_wconv = T.buf("wconv")
                for nm_, src_ in (("gates", w_gates_d), ("mo", w_mo_d), ("out", w_out_d), ("pm", w_pm_d)):
                    dma("gpsimd", lambda e: e.dma_start(out=wC16[nm_][:, :, :], in_=src_[:, :, :]), lane=B_wconv)
                for wi, wsrc in enumerate((wg_d, wu_d, wd_d)):
                    for r8 in range(8):
                        o_ = w16_all[r8 * 512:(r8 + 1) * 512, wi * 4096:(wi + 1) * 4096].rearrange("r (a n) -> r a n", n=2048)
                        i_ = wsrc[r8 * 512:(r8 + 1) * 512, :].rearrange("r (a n) -> r a n", n=2048)
                        dma("gpsimd", lambda e: e.dma_start(out=o_, in_=i_), lane=B_wconv)
                VT = [sbt(cB, "VT%d" % i, [128, 64, 128], BF16) for i in range(2)]
                B_VT = [T.buf("VT0"), T.buf("VT1")]
                op("gpsimd", lambda e: e.memset(VT[0][:, :, 64:128], 1.0), writes=[B_VT[0]])
                op("gpsimd", lambda e: e.memset(VT[1][:, :, 0:64], 1.0), writes=[B_VT[1]])
                QT = [sbt(cB, "QT%d" % i, [128, 512], BF16) for i in range(2)]
                B_QT = [T.buf("QT0"), T.buf("QT1")]
                tq = [sbt(cB, "tq%d" % i, [128, 512], F32) for i in range(2)]
                B_tq = [T.buf("tq0"), T.buf("tq1")]
                NPT = 3
                PT2 = [sbt(cB, "PT2_%d" % i, [128, 2, 512], BF16) for i in range(NPT)]
                B_PT2 = [T.buf("PT2_%d" % i) for i in range(NPT)]
                rden = sbt(cB, "rden", [128, 512], F32)
                B_rden = T.buf("rden")
                PS2 = [pst(cB, "PS2_%d" % i, [128, 2, 512], F32) for i in range(2)]
                B_PS2h = [[T.buf("PS2_%d_%d" % (i, hh)) for hh in range(2)] for i in range(2)]
                P = {i: pst(cB, "PB%d" % i, [128, 512], F32) for i in (3, 4, 5, 6)}
                B_P = {i: T.buf("PB%d" % i) for i in (3, 4, 5, 6)}
                brot = [0]
                bmode = ["all"]
                BUILD_VIEWS = [(PS2[0][:, 0, :], B_PS2h[0][0]), (PS2[0][:, 1, :], B_PS2h[0][1]),
                               (PS2[1][:, 0, :], B_PS2h[1][0]), (PS2[1][:, 1, :], B_PS2h[1][1]),
                               (P[6][:], B_P[6])]

                def build_kv_items(h):
                    items = []
                    for kb in range(16):
                        items.append(lambda kb=kb: build_k_item(h, kb))
                    for grp in range(8):
                        items.append(lambda grp=grp: build_v_item(h, grp))
                    return items

                def build_k_item(h, kb):
                    kb_ = h % 2
                    if True:
                        pv_, Bp = BUILD_VIEWS[brot[0] % 5] if bmode[0] == "all" else BUILD_VIEWS[4]
                        brot[0] += 1
                        for m in range(2):
                            mm(pv_[64:128, :], w_uk[:, m, h, :], CKVN[:, m, kb * 512:(kb + 1) * 512], m == 0, m == 1,
                               [B_wB, B_ckvn], [Bp])
                        if False:
                            pass
                        else:
                            op("vector", lambda e: e.tensor_copy(out=KT[kb_][64:128, kb * 512:(kb + 1) * 512],
                                                                 in_=pv_[64:128, :]),
                               reads=[Bp], writes=[B_KThi[kb_]])

                def build_v_item(h, grp):
                    kb_ = h % 2
                    voff = 0 if h % 2 == 0 else 64
                    if True:
                        pv_, Bp = BUILD_VIEWS[brot[0] % 5] if bmode[0] == "all" else BUILD_VIEWS[4]
                        brot[0] += 1
                        pv = pv_.rearrange("p (j v) -> p j v", j=8)
                        for j in range(8):
                            kt = grp * 8 + j
                            for m in range(2):
                                mm(pv[:, j, :], CKVN[:, m, kt * 128:(kt + 1) * 128], w_uv[:, m, h, :], m == 0, m == 1,
                                   [B_wB, B_ckvn], [Bp])
                        if True:
                            op("vector", lambda e: e.tensor_copy(out=VT[kb_][:, grp * 8:(grp + 1) * 8, voff:voff + 64],
                                                                 in_=pv),
                               reads=[Bp], writes=[B_VT[kb_]])

                def build_q(step):
                    h, Q = step // 8, step % 8
                    s = step % 2
                    dma("sync", lambda e: e.dma_start(out=tq[s][:], in_=tblq_d[:, Q * 512:(Q + 1) * 512]),
                        writes=[B_tq[s]], lane=B_tq[s])
                    for kc in range(3):
                        mm(P[5][:], w_uq[:, kc, h, :], CQN[:, kc, Q * 512:(Q + 1) * 512], kc == 0, kc == 2,
                           [B_wB, B_cqn], [B_P[5]])
                    op("vector", lambda e: e.tensor_tensor(out=QT[s][:], in0=P[5][:], in1=tq[s][:], op=ALU.mult),
                       reads=[B_P[5], B_tq[s]], writes=[B_QT[s]])

                unit_ctr = [0]

                def attn_step(step, fill):
                    h, Q = step // 8, step % 8
                    s = step % 2
                    kb_ = h % 2
                    po = 3 + (step % 2)
                    pair = h // 2
                    nfull = 8 * Q
                    units = []
                    for u in range(4 * Q):
                        units.append((False, ((2 * u, 0), (2 * u + 1, 0))))
                    for jp in range(4):
                        je, jo = 2 * jp, 2 * jp + 1
                        units.append((True, ((nfull + je, 64 * je), (nfull + jo, 64 * jo))))
                    total_pv = 2 * len(units)
                    if h % 2 == 0:
                        nlo, dlo = 0, 64
                    else:
                        nlo, dlo = 64, 0

                    def finalize():
                        op("vector", lambda e: e.reciprocal(out=rden[nlo:nlo + 64, :], in_=P[po][dlo:dlo + 64, :]),
                           reads=[B_P[po]], writes=[B_rden])
                        op("vector", lambda e: e.tensor_tensor(out=OT[nlo:nlo + 64, pair, Q * 512:(Q + 1) * 512],
                                                               in0=P[po][nlo:nlo + 64, :], in1=rden[nlo:nlo + 64, :],
                                                               op=ALU.mult),
                           reads=[B_P[po], B_rden], writes=[B_OT[pair]])

                    ctx = {"npv": 0, "total": total_pv, "po": po, "kb": kb_, "fin": finalize}

                    def emit_qk(unit):
                        diag, tl = unit
                        ui = unit_ctr[0]
                        unit_ctr[0] += 1
                        ps, pt = ui % 2, ui % NPT
                        c0 = tl[0][1]
                        for hh, (k, c) in enumerate(tl):
                            mm(PS2[ps][:, hh, c:512], KT[kb_][:, k * 128:(k + 1) * 128], QT[s][:, c:512], True, True,
                               [B_KTlo[kb_], B_KThi[kb_], B_QT[s]], [B_PS2h[ps][hh]])
                        op("scalar", lambda e: e.activation(out=PT2[pt][:, :, c0:512], in_=PS2[ps][:, :, c0:512],
                                                            func=AF.Exp),
                           reads=[B_PS2h[ps][0], B_PS2h[ps][1]], writes=[B_PT2[pt]])
                        if diag:
                            for hh, (k, c) in enumerate(tl):
                                op("gpsimd", lambda e: e.memset(PT2[pt][64:128, hh, c:c + 32], 0.0),
                                   writes=[B_PT2[pt]])
                        return (tl, pt, ctx)

                    for ui_, unit in enumerate(units):
                        GP.append(emit_qk(unit))
                        if len(GP) > 2:
                            emit_pv(GP.pop(0))
                        if fill and ui_ % 6 == 5:
                            fill.pop(0)()

                GP = []

                def emit_pv(rec):
                    tl, pt, ctx = rec
                    po_, kbx = ctx["po"], ctx["kb"]
                    for hh, (k, c) in enumerate(tl):
                        ctx["npv"] += 1
                        mm(P[po_][:, c:512], VT[kbx][:, k, :], PT2[pt][:, hh, c:512], ctx["npv"] == 1,
                           ctx["npv"] == ctx["total"], [B_VT[kbx], B_PT2[pt]], [B_P[po_]])
                    if ctx["npv"] == ctx["total"]:
                        ctx["fin"]()

                for it in build_kv_items(0):
                    it()
                bmode[0] = "fill"
                build_q(0)
                NH = NHEADS_RUN
                for h in range(NH):
                    fill = build_kv_items(h + 1) if h + 1 < NH else []
                    for Q in range(8):
                        step = h * 8 + Q
                        if step + 1 < NH * 8:
                            build_q(step + 1)
                        attn_step(step, fill)
                    while fill:
                        fill.pop(0)()
                while GP:
                    emit_pv(GP.pop(0))
                if DEBUG:
                    dma("sync", lambda e: e.dma_start(out=d_kt[:, :], in_=KT[1][:]), reads=[B_KTlo[1], B_KThi[1]],
                        lane=B_KTlo[1])
                    dma("sync", lambda e: e.dma_start(out=d_ot[:, :, :], in_=OT[:]), reads=B_OT, lane=B_OT[0])
                T.barrier()
                if STOP == "B":
                    return

        with contextlib.ExitStack() as cC:
            w_gates = sbt(cC, "w_gates", [128, 8, 2048], BF16)
            w_mo = sbt(cC, "w_mo", [128, 4, 1024], BF16)
            w_out = sbt(cC, "w_out", [128, 8, 1024], BF16)
            w_pm = sbt(cC, "w_pm", [128, 4, 256], BF16)
            psc = sbt(cC, "psc", [128, 8], F32)
            lng = sbt(cC, "lng", [128, D], F32)
            lnb = sbt(cC, "lnb", [128, D], F32)
            w_r = sbt(cC, "w_r", [128, 8, 36], F32)
            brb = sbt(cC, "brb", [128, 36], F32)
            B_wC = T.buf("wC")
            B_wG = T.buf("wG")
            dma("sync", lambda e: e.dma_start(out=w_gates[:], in_=wC16["gates"][:, :, :]), writes=[B_wG], lane=B_wG)
            for t_, d_ in ((w_mo, wC16["mo"]), (w_out, wC16["out"]), (w_pm, wC16["pm"])):
                dma("sync", lambda e, t_=t_, d_=d_: e.dma_start(out=t_[:], in_=d_[:, :, :]), writes=[B_wC], lane=B_wC)
            dma("sync", lambda e: e.dma_start(out=psc[:], in_=psc_d[:, :]), writes=[B_wC], lane=B_wC)
            dma("sync", lambda e: e.dma_start(out=lng[:], in_=ln1g_d.broadcast_to([128, D])), writes=[B_wC], lane=B_wC)
            dma("sync", lambda e: e.dma_start(out=lnb[:], in_=ln1b_d.broadcast_to([128, D])), writes=[B_wC], lane=B_wC)
            dma("sync", lambda e: e.dma_start(out=w_r[:], in_=w_r_d[:, :, :]), writes=[B_wC], lane=B_wC)
            dma("sync", lambda e: e.dma_start(out=brb[:], in_=b_r_d.broadcast_to([128, 36])), writes=[B_wC], lane=B_wC)
            _xbo = sbt(cC, "xbo", [128, 8, 512], BF16)
            xbo = [_xbo, _xbo]
            _bx = T.buf("xbo")
            B_xbo = [_bx, _bx]
            _plb = sbt(cC, "PLb", [128, 4, 512], BF16)
            PLb = [_plb, _plb]
            _bp = T.buf("PLb")
            B_PLb = [_bp, _bp]
            Gt = sbt(cC, "Gt", [128, 16, 512], BF16)
            B_G = [T.buf("G%d" % i) for i in range(16)]
            MT = [sbt(cC, "MT%d" % i, [128, 8, 512], BF16) for i in range(2)]
            B_MT = [T.buf("MT0"), T.buf("MT1")]
            At2 = [sbt(cC, "At%d" % i, [128, 512], F32) for i in range(2)]
            Bt2 = [sbt(cC, "Bt%d" % i, [128, 512], F32) for i in range(2)]
            B_At2 = [T.buf("At0"), T.buf("At1")]
            B_Bt2 = [T.buf("Bt0"), T.buf("Bt1")]
            mhalf = sbt(cC, "mhalf", [128, 1], F32)
            B_mh = T.buf("mhalf")
            op("gpsimd", lambda e: e.memset(mhalf[:], -0.5), writes=[B_mh])
            xo = [sbt(cC, "xo%d" % i, [128, D], F32) for i in range(2)]
            B_xo = [T.buf("xo0"), T.buf("xo1")]
            R1s = [sbt(cC, "R1_%d" % i, [128, D], F32) for i in range(2)]
            B_R1s = [T.buf("R1_0"), T.buf("R1_1")]
            H1 = [sbt(cC, "H1%d" % i, [128, D], F32) for i in range(3)]
            B_H1 = [T.buf("H10"), T.buf("H11"), T.buf("H12")]
            H1T = sbt(cC, "H1T", [128, 8, 128], F32)
            B_H1T = [T.buf("H1Ta"), T.buf("H1Tb")]
            P = [pst(cC, "PC%d" % i, [128, 512], F32) for i in range(8)]
            B_P = [T.buf("PC%d" % i) for i in range(8)]

            LNT = {}
            for nm_ in ("st", "mv", "rstd", "nbias", "veps"):
                shp = {"st": [128, 2, 6], "mv": [128, 2], "rstd": [128, 1], "nbias": [128, 1], "veps": [128, 1]}[nm_]
                LNT[nm_] = [sbt(cC, "ln_%s%d" % (nm_, i), shp, F32) for i in range(2)]
                LNT["B_" + nm_] = [T.buf("ln_%s%d" % (nm_, i)) for i in range(2)]

            def ln_p1a(i, src, B_src, B_gb):
                k = i % 2
                st, mv, rstd, veps = LNT["st"][k], LNT["mv"][k], LNT["rstd"][k], LNT["veps"][k]
                B_st, B_mv, B_rstd, B_veps = LNT["B_st"][k], LNT["B_mv"][k], LNT["B_rstd"][k], LNT["B_veps"][k]
                for c in range(2):
                    op("vector", lambda e: e.bn_stats(out=st[:, c, :], in_=src[:, c * 512:(c + 1) * 512]),
                       reads=[B_src], writes=[B_st])
                op("vector", lambda e: e.bn_aggr(out=mv[:, :], in_=st[:, :, :].rearrange("p a b -> p (a b)")),
                   reads=[B_st], writes=[B_mv])
                op("gpsimd", lambda e: e.tensor_scalar(out=veps[:], in0=mv[:, 1:2], scalar1=EPS, scalar2=None, op0=ALU.add),
                   reads=[B_mv], writes=[B_veps])
                op("gpsimd", lambda e: e.tensor_tensor(out=rstd[:], in0=veps[:], in1=mhalf[:], op=ALU.pow),
                   reads=[B_veps, B_mh], writes=[B_rstd])

            def ln_p1b(i, src, B_src, dst, B_dst):
                k = i % 2
                mv, rstd, nbias = LNT["mv"][k], LNT["rstd"][k], LNT["nbias"][k]
                B_mv, B_rstd, B_nbias = LNT["B_mv"][k], LNT["B_rstd"][k], LNT["B_nbias"][k]
                op("vector", lambda e: e.scalar_tensor_tensor(out=nbias[:], in0=mv[:, 0:1], scalar=-1.0, in1=rstd[:],
                                                              op0=ALU.mult, op1=ALU.mult),
                   reads=[B_mv, B_rstd], writes=[B_nbias])
                op("scalar", lambda e: e.activation(out=dst[:], in_=src[:], func=AF.Identity, scale=rstd[:, 0:1],
                                                    bias=nbias[:, 0:1]),
                   reads=[B_src, B_nbias, B_rstd], writes=[B_dst])

            def ln_p2(dst, B_dst, g_t, b_t, B_gb):
                op("vector", lambda e: e.tensor_tensor(out=dst[:], in0=dst[:], in1=g_t[:], op=ALU.mult),
                   reads=[B_dst, B_gb], writes=[B_dst])
                op("vector", lambda e: e.tensor_tensor(out=dst[:], in0=dst[:], in1=b_t[:], op=ALU.add),
                   reads=[B_dst, B_gb], writes=[B_dst])

            def loadC_x(Q):
                dma("gpsimd", lambda e: e.dma_start(out=xbo[0][:], in_=xT_own[:, :, Q * 512:(Q + 1) * 512]),
                    writes=[B_xbo[0]], lane=B_xbo[0])

            def loadC_p(Q):
                dma("sync", lambda e: e.dma_start(out=PLb[0][:], in_=poolT_d[:, :, Q * 512:(Q + 1) * 512]),
                    writes=[B_PLb[0]], lane=B_PLb[0])

            def stage1a(i, ms):
                tt = i % 4
                hs = i % 2
                if i == 0:
                    dma("sync", lambda e: e.dma_start(out=xo[0][:], in_=x_own[0:128, :]),
                        writes=[B_xo[0]], lane=B_xo[0])
                if i + 1 < NT:
                    hn = (i + 1) % 2
                    dma("sync", lambda e: e.dma_start(out=xo[hn][:], in_=x_own[(i + 1) * 128:(i + 2) * 128, :]),
                        writes=[B_xo[hn]], lane=B_xo[hn])
                for h2 in range(2):
                    for dc in range(8):
                        mm(P[4 + h2][:], MT[ms][:, dc, tt * 128:(tt + 1) * 128], w_out[:, dc, h2 * 512:(h2 + 1) * 512],
                           dc == 0, dc == 7, [B_MT[ms], B_wC], [B_P[4 + h2]])
                R1, B_R1 = R1s[i % 2], B_R1s[i % 2]
                for h2 in range(2):
                    op("vector", lambda e: e.scalar_tensor_tensor(out=R1[:, h2 * 512:(h2 + 1) * 512],
                                                                  in0=xo[hs][:, h2 * 512:(h2 + 1) * 512],
                                                                  scalar=ALPHA, in1=P[4 + h2][:],
                                                                  op0=ALU.mult, op1=ALU.add),
                       reads=[B_xo[hs], B_P[4 + h2]], writes=[B_R1])
                ln_p1a(i, R1, B_R1, B_wC)

            def stage1b(i):
                h3 = i % 3
                ln_p1b(i, R1s[i % 2], B_R1s[i % 2], H1[h3], B_H1[h3])

            def stage1c(i):
                h3 = i % 3
                ln_p2(H1[h3], B_H1[h3], lng, lnb, B_wC)
                dma("sync", lambda e: e.dma_start(out=h1_d[i * 128:(i + 1) * 128, :], in_=H1[h3][:]),
                    reads=[B_H1[h3]], lane=B_H1[h3])

            def stage2a(i):
                hs = i % 3
                for hb in range(2):
                    ptr = P[6 + hb][:, :].rearrange("p (a t) -> p a t", a=4)
                    for a in range(4):
                        dc = hb * 4 + a
                        op("tensor", lambda e: e.transpose(out=ptr[:, a, :], in_=H1[hs][:, dc * 128:(dc + 1) * 128],
                                                           identity=identf[:]),
                           reads=[B_H1[hs], B_const], writes=[B_P[6 + hb]])
                    op("scalar", lambda e: e.activation(out=H1T[:, hb * 4:(hb + 1) * 4, :], in_=ptr, func=AF.Copy),
                       reads=[B_P[6 + hb]], writes=[B_H1T[hb]])
                for dc in range(8):
                    mm(P[2][:, 0:36], H1T[:, dc, :], w_r[:, dc, :], dc == 0, dc == 7,
                       [B_H1T[dc // 4], B_wC], [B_P[2]])

            def stage2b(i):
                op("vector", lambda e: e.tensor_tensor(out=LG[:, i, :], in0=P[2][:, 0:36], in1=brb[:], op=ALU.add),
                   reads=[B_P[2], B_wC], writes=[B_LG])

            gctr = [0]

            def gate_chunk(Q, m):
                s = 0
                pg = gctr[0] % 2
                gctr[0] += 1
                for c in range(8):
                    mm(P[pg][:], w_gates[:, c, m * 128:(m + 1) * 128], xbo[s][:, c, :], c == 0, c == 7,
                       [B_wG, B_xbo[s]], [B_P[pg]])
                op("scalar", lambda e: e.activation(out=Gt[:, m, :], in_=P[pg][:], func=AF.Sigmoid),
                   reads=[B_P[pg]], writes=[B_G[m]])

            def merge_dc(Q, dc):
                s = 0
                g, hh = dc // 2, dc % 2
                py_, pm_ = (2, 3) if dc % 2 == 0 else (6, 7)
                At, Bt, B_At, B_Bt = At2[dc % 2], Bt2[dc % 2], B_At2[dc % 2], B_Bt2[dc % 2]
                mm(P[py_][:], w_pm[:, g, hh * 128:(hh + 1) * 128], PLb[s][:, g, :], True, True,
                   [B_wC, B_PLb[s]], [B_P[py_]])
                for pr in range(4):
                    mm(P[pm_][:], w_mo[:, pr, dc * 128:(dc + 1) * 128], OT[:, pr, Q * 512:(Q + 1) * 512],
                       pr == 0, pr == 3, [B_wC, B_OT[pr]], [B_P[pm_]])
                op("vector", lambda e: e.scalar_tensor_tensor(out=At[:], in0=P[py_][:], scalar=psc[:, dc:dc + 1],
                                                              in1=Gt[:, dc, :], op0=ALU.mult, op1=ALU.mult),
                   reads=[B_P[py_], B_wC, B_G[dc]], writes=[B_At])
                op("vector", lambda e: e.tensor_tensor(out=Bt[:], in0=P[pm_][:], in1=Gt[:, 8 + dc, :], op=ALU.mult),
                   reads=[B_P[pm_], B_G[8 + dc]], writes=[B_Bt])
                op("vector", lambda e: e.tensor_tensor(out=MT[Q % 2][:, dc, :], in0=At[:], in1=Bt[:], op=ALU.add),
                   reads=[B_At, B_Bt], writes=[B_MT[Q % 2]])

            loadC_x(0)
            loadC_p(0)
            pending2 = []
            for m in range(16):
                gate_chunk(0, m)
            loadC_x(1)
            for Q in range(8):
                for dc in range(8):
                    merge_dc(Q, dc)
                    if Q + 1 < 8:
                        gate_chunk(Q + 1, dc)
                        gate_chunk(Q + 1, 8 + dc)
                if Q + 1 < 8:
                    loadC_p(Q + 1)
                if Q + 2 < 8:
                    loadC_x(Q + 2)
                for tt in range(4):
                    i = Q * 4 + tt
                    stage1a(i, Q % 2)
                    p2 = pending2.pop(0) if len(pending2) > 1 else None
                    if p2 is not None:
                        stage2a(p2)
                    if i >= 1:
                        stage1c(i - 1)
                    stage1b(i)
                    if p2 is not None:
                        stage2b(p2)
                    pending2.append(i)
            stage1c(NT - 1)
            while pending2:
                p2 = pending2.pop(0)
                stage2a(p2)
                stage2b(p2)
            T.barrier()

        with contextlib.ExitStack() as cC:
            P = [pst(cC, "PR%d" % i, [128, 512], F32) for i in range(4)]
            B_P = [T.buf("PR%d" % i) for i in range(4)]
            B_wC = T.buf("wC2")

            def rt(name, shape, dt=F32):
                return sbt(cC, name, shape, dt)
            B_r = T.buf("rtmp")

            def vop(fn, eng="vector"):
                return op(eng, fn, reads=[B_r, B_LG, B_const, B_wC], writes=[B_r])

            GL = LG[:, :, 0:4]
            gmax = rt("gmax", [128, NT])
            ohg = rt("ohg", [128, NT, 4])
            gd = rt("gd", [128, NT, 4])
            gsum = rt("gsum", [128, NT])
            gw = rt("gw", [128, NT])
            tmp4 = rt("tmp4", [128, NT, 4, 8])
            SEL = rt("SEL", [128, NT, 8])
            SEL2 = rt("SEL2", [128, NT, 8])
            m1 = rt("m1", [128, NT])
            m2 = rt("m2", [128, NT])
            oh1 = rt("oh1", [128, NT, 8])
            oh2 = rt("oh2", [128, NT, 8])
            rr = rt("rr", [128, NT])
            rinv = rt("rinv", [128, NT])
            E1 = rt("E1", [128, NT, 4, 8])
            E2 = rt("E2", [128, NT, 4, 8])
            EEb = rt("EEb", [128, NT, 32], BF16)
            lstr = rt("lstr", [128, 128], BF16)
            CUM = rt("CUM", [128, NT, 32])
            TOT = rt("TOT", [128, NT, 32])
            TP = rt("TP", [128, NT, 32])
            cnt = rt("cnt", [128, 32])
            b128i = rt("b128i", [128, NBLK], I32)
            b128 = rt("b128", [128, NBLK])
            pioi = rt("pioi", [128, 1], I32)
            pio = rt("pio", [128, 1])
            cmpk = rt("cmpk", [128, 32, 32])
            padded = rt("padded", [128, 32])
            sc0 = rt("sc0", [128, 32])
            sc1 = rt("sc1", [128, 32])
            pstart = rt("pstart", [128, 32])
            POS = rt("POS", [128, NT, 32])
            D1f = rt("D1f", [128, NT])
            D2f = rt("D2f", [128, NT])
            cmpb = rt("cmpb", [128, NBLK, 32])
            BE = rt("BE", [128, NBLK])

            dma("gpsimd", lambda e: e.dma_start(out=lstr[:], in_=lstrict_d[:, :]), writes=[B_r], lane=B_r)
            vop(lambda e: e.iota(b128i[:], pattern=[[BLKR, NBLK]], base=0, channel_multiplier=0), "gpsimd")
            vop(lambda e: e.iota(pioi[:], pattern=[[0, 1]], base=0, channel_multiplier=1), "gpsimd")
            vop(lambda e: e.tensor_copy(out=b128[:], in_=b128i[:]))
            vop(lambda e: e.tensor_copy(out=pio[:], in_=pioi[:]))

            def bc3(ap2, n):
                return ap2.unsqueeze(2).broadcast_to([128, NT, n])

            vop(lambda e: e.tensor_reduce(out=gmax[:], in_=GL, axis=AX.X, op=ALU.max))
            vop(lambda e: e.tensor_tensor(out=ohg[:], in0=GL, in1=bc3(gmax[:], 4), op=ALU.is_equal))
            vop(lambda e: e.tensor_tensor(out=gd[:], in0=GL, in1=bc3(gmax[:], 4), op=ALU.subtract))
            vop(lambda e: e.activation(out=gd[:], in_=gd[:], func=AF.Exp), "scalar")
            vop(lambda e: e.tensor_reduce(out=gsum[:], in_=gd[:], axis=AX.X, op=ALU.add))
            vop(lambda e: e.reciprocal(out=gw[:], in_=gsum[:]))
            EL = LG[:, :, 4:36].rearrange("p t (g j) -> p t g j", g=4)
            vop(lambda e: e.tensor_tensor(out=tmp4[:], in0=EL, in1=ohg[:].unsqueeze(3).broadcast_to([128, NT, 4, 8]),
                                          op=ALU.mult))
            vop(lambda e: e.tensor_reduce(out=SEL[:], in_=tmp4[:].rearrange("p t g j -> p t j g"), axis=AX.X, op=ALU.add))
            vop(lambda e: e.tensor_reduce(out=m1[:], in_=SEL[:], axis=AX.X, op=ALU.max))
            vop(lambda e: e.tensor_tensor(out=oh1[:], in0=SEL[:], in1=bc3(m1[:], 8), op=ALU.is_equal))
            vop(lambda e: e.scalar_tensor_tensor(out=SEL2[:], in0=oh1[:], scalar=-1.0e30, in1=SEL[:],
                                                 op0=ALU.mult, op1=ALU.add))
            vop(lambda e: e.tensor_reduce(out=m2[:], in_=SEL2[:], axis=AX.X, op=ALU.max))
            vop(lambda e: e.tensor_tensor(out=oh2[:], in0=SEL2[:], in1=bc3(m2[:], 8), op=ALU.is_equal))
            vop(lambda e: e.tensor_tensor(out=rr[:], in0=m2[:], in1=m1[:], op=ALU.subtract))
            vop(lambda e: e.activation(out=rr[:], in_=rr[:], func=AF.Exp), "scalar")
            hinR = [rt("hinR%d" % i, [128, D]) for i in range(4)]
            B_hinR = [T.buf("hinR%d" % i) for i in range(4)]
            hbA = rt("hbA", [128, NT, D], BF16)
            B_hbA = [T.buf("hbA%d" % i) for i in range(NT)]
            B_sc = [T.buf("sc%d" % i) for i in range(4)]
            for i in range(NT):
                s4 = i % 4
                dma("sync", lambda e: e.dma_start(out=hinR[s4][:], in_=h1_d[i * 128:(i + 1) * 128, :]),
                    writes=[B_hinR[s4]], lane=B_hinR[s4])
                op("scalar", lambda e: e.activation(out=hbA[:, i, :], in_=hinR[s4][:], func=AF.Copy),
                   reads=[B_hinR[s4]], writes=[B_hbA[i]])
            vop(lambda e: e.tensor_scalar(out=rinv[:], in0=rr[:], scalar1=1.0, scalar2=None, op0=ALU.add))
            vop(lambda e: e.reciprocal(out=rinv[:], in_=rinv[:]))
            vop(lambda e: e.tensor_tensor(out=G1[:], in0=gw[:], in1=rinv[:], op=ALU.mult))
            vop(lambda e: e.tensor_tensor(out=G2[:], in0=G1[:], in1=rr[:], op=ALU.mult))
            ohg4 = ohg[:].unsqueeze(3).broadcast_to([128, NT, 4, 8])
            vop(lambda e: e.tensor_tensor(out=E1[:], in0=ohg4, in1=oh1[:].unsqueeze(2).broadcast_to([128, NT, 4, 8]),
                                          op=ALU.mult))
            vop(lambda e: e.tensor_tensor(out=E2[:], in0=ohg4, in1=oh2[:].unsqueeze(2).broadcast_to([128, NT, 4, 8]),
                                          op=ALU.mult))
            E1v = E1[:].rearrange("p t g j -> p t (g j)")
            E2v = E2[:].rearrange("p t g j -> p t (g j)")
            vop(lambda e: e.tensor_tensor(out=EEb[:], in0=E1v, in1=E2v, op=ALU.add))
            for hb in range(2):
                rhs = EEb[:, hb * 16:(hb + 1) * 16, :].rearrange("p t e -> p (t e)")
                op("tensor", lambda e: e.matmul(P[hb][:], lhsT=lstr[:], rhs=rhs, start=True, stop=True),
                   reads=[B_r], writes=[B_P[hb]])
                op("tensor", lambda e: e.matmul(P[2 + hb][:], lhsT=onesb[:], rhs=rhs, start=True, stop=True),
                   reads=[B_r, B_const], writes=[B_P[2 + hb]])
            for hb in range(2):
                op("vector", lambda e: e.tensor_copy(out=CUM[:, hb * 16:(hb + 1) * 16, :].rearrange("p t e -> p (t e)"),
                                                     in_=P[hb][:]), reads=[B_P[hb], B_r], writes=[B_r])
                op("vector", lambda e: e.tensor_copy(out=TOT[:, hb * 16:(hb + 1) * 16, :].rearrange("p t e -> p (t e)"),
                                                     in_=P[2 + hb][:]), reads=[B_P[2 + hb], B_r], writes=[B_r])
            vop(lambda e: e.memset(TP[:, 0, :], 0.0))
            for i in range(1, NT):
                vop(lambda e: e.tensor_tensor(out=TP[:, i, :], in0=TP[:, i - 1, :], in1=TOT[:, i - 1, :], op=ALU.add))
            vop(lambda e: e.tensor_tensor(out=cnt[:], in0=TP[:, NT - 1, :], in1=TOT[:, NT - 1, :], op=ALU.add))
            vop(lambda e: e.tensor_tensor(out=cmpk[:], in0=cnt[:].unsqueeze(2).broadcast_to([128, 32, 32]),
                                          in1=b128[:, 0:32].unsqueeze(1).broadcast_to([128, 32, 32]), op=ALU.is_gt))
            vop(lambda e: e.tensor_reduce(out=padded[:], in_=cmpk[:], axis=AX.X, op=ALU.add))
            vop(lambda e: e.tensor_scalar(out=padded[:], in0=padded[:], scalar1=float(BLKR), scalar2=None, op0=ALU.mult))
            vop(lambda e: e.tensor_copy(out=sc0[:], in_=padded[:]))
            cur, nxt = sc0, sc1
            for sh in (1, 2, 4, 8, 16):
                vop(lambda e: e.tensor_copy(out=nxt[:, 0:sh], in_=cur[:, 0:sh]))
                vop(lambda e: e.tensor_tensor(out=nxt[:, sh:32], in0=cur[:, sh:32], in1=cur[:, 0:32 - sh], op=ALU.add))
                cur, nxt = nxt, cur
            pend_t = cur
            vop(lambda e: e.tensor_tensor(out=pstart[:], in0=pend_t[:], in1=padded[:], op=ALU.subtract))
            vop(lambda e: e.tensor_tensor(out=POS[:], in0=CUM[:], in1=TP[:], op=ALU.add))
            vop(lambda e: e.tensor_tensor(out=POS[:], in0=POS[:], in1=pstart[:].unsqueeze(1).broadcast_to([128, NT, 32]),
                                          op=ALU.add))
            vop(lambda e: e.tensor_tensor(out=CUM[:], in0=POS[:], in1=E1v, op=ALU.mult))
            vop(lambda e: e.tensor_reduce(out=D1f[:], in_=CUM[:], axis=AX.X, op=ALU.add))
            vop(lambda e: e.tensor_tensor(out=CUM[:], in0=POS[:], in1=E2v, op=ALU.mult))
            vop(lambda e: e.tensor_reduce(out=D2f[:], in_=CUM[:], axis=AX.X, op=ALU.add))
            op("vector", lambda e: e.tensor_copy(out=D1i[:], in_=D1f[:]), reads=[B_r], writes=[B_rt])
            op("vector", lambda e: e.tensor_copy(out=D2i[:], in_=D2f[:]), reads=[B_r], writes=[B_rt])
            vop(lambda e: e.tensor_tensor(out=cmpb[:], in0=pend_t[:].unsqueeze(1).broadcast_to([128, NBLK, 32]),
                                          in1=b128[:].unsqueeze(2).broadcast_to([128, NBLK, 32]), op=ALU.is_le))
            vop(lambda e: e.tensor_reduce(out=BE[:], in_=cmpb[:], axis=AX.X, op=ALU.add))
            vop(lambda e: e.tensor_scalar(out=BE[:], in0=BE[:], scalar1=128.0, scalar2=None, op0=ALU.mult))
            vop(lambda e: e.tensor_scalar(out=BE[:], in0=BE[:], scalar1=pio[:, 0:1], scalar2=None, op0=ALU.add))
            op("vector", lambda e: e.tensor_copy(out=IDXW[:], in_=BE[:]), reads=[B_r], writes=[B_rt])
            for i in range(NT):
                for Di in (D1i, D2i):
                    dma("gpsimd", lambda e: e.indirect_dma_start(
                        out=xs_d[:, :], out_offset=bass.IndirectOffsetOnAxis(ap=Di[:, i:i + 1], axis=0),
                        in_=hbA[:, i, :], in_offset=None), reads=[B_hbA[i], B_rt], lane=B_sc[i % 4])
            if DEBUG:
                dma("sync", lambda e: e.dma_start(out=d_lg[:, :, :], in_=LG[:]), reads=[B_LG], lane=B_LG)
                dma("sync", lambda e: e.dma_start(out=d_rt[:, 0, :], in_=D1f[:]), reads=[B_r], lane=B_r)
                dma("sync", lambda e: e.dma_start(out=d_rt[:, 1, :], in_=D2f[:]), reads=[B_r], lane=B_r)
                dma("sync", lambda e: e.dma_start(out=d_rt[:, 2, :], in_=G1[:]), reads=[B_r], lane=B_r)
                dma("sync", lambda e: e.dma_start(out=d_rt[:, 3, :], in_=G2[:]), reads=[B_r], lane=B_r)
                dma("sync", lambda e: e.dma_start(out=d_idx[:, :], in_=IDXW[:]), reads=[B_rt], lane=B_rt)
            T.barrier()
            if STOP == "C":
                return

        with contextlib.ExitStack() as cD:
            hin = [sbt(cD, "hin%d" % i, [128, D], F32) for i in range(4)]
            B_hin = [T.buf("hin%d" % i) for i in range(4)]
            with contextlib.ExitStack() as cD1:
                NW = 4
                WALL = [sbt(cD1, "WALL%d" % i, [128, 3 * 4096], BF16) for i in range(NW)]
                WG = [w_[:, 0:4096].rearrange("p (c n) -> p c n", c=8) for w_ in WALL]
                WU = [w_[:, 4096:8192].rearrange("p (c n) -> p c n", c=8) for w_ in WALL]
                WD = [w_[:, 8192:12288].rearrange("p (f n) -> p f n", f=4) for w_ in WALL]
                B_Wgu = [T.buf("W%d" % i) for i in range(NW)]
                B_Wd = B_Wgu
                NX = 6
                xsb = [sbt(cD1, "xsb%d" % i, [128, D], BF16) for i in range(NX)]
                B_xsb = [T.buf("xsb%d" % i) for i in range(NX)]
                XBT = [sbt(cD1, "XBT%d" % i, [128, 8, 128], BF16) for i in range(2)]
                B_XBT = [T.buf("XBT0"), T.buf("XBT1")]
                SG = [sbt(cD1, "SG%d" % i, [128, 512], F32) for i in range(2)]
                B_SG = [T.buf("SG0"), T.buf("SG1")]
                HT = [sbt(cD1, "HT%d" % i, [128, 4, 128], BF16) for i in range(2)]
                B_HT = [T.buf("HT0"), T.buf("HT1")]
                YB = [sbt(cD1, "YB%d" % i, [128, D], F32) for i in range(2)]
                B_YB = [T.buf("YB0"), T.buf("YB1")]
                PTB = pst(cD1, "PTB", [128, 8, 128], BF16)
                B_PTB = T.buf("PTB")
                HID = [sbt(cD1, "HID%d" % i, [128, 512], BF16) for i in range(2)]
                B_HID = [T.buf("HID0"), T.buf("HID1")]
                P = [pst(cD1, "PD%d" % i, [128, 512], F32) for i in range(6)]
                B_P = [T.buf("PD%d" % i) for i in range(6)]
                PTH = pst(cD1, "PTH", [128, 4, 128], BF16)
                B_PTH = T.buf("PTH")

                bc_reg = nc.gpsimd.to_reg(4095)

                def wload(b):
                    w = b % NW
                    dma("gpsimd", lambda e: e.indirect_dma_start(
                        out=WALL[w][:], out_offset=None, in_=w16_all[:, :],
                        in_offset=bass.IndirectOffsetOnAxis(ap=IDXW[:, b:b + 1], axis=0),
                        bounds_check=bc_reg, oob_is_err=False),
                        reads=[B_rt], writes=[B_Wgu[w]], lane=B_Wgu[w])

                def xload(b):
                    s = b % NX
                    dma("sync", lambda e: e.dma_start(out=xsb[s][:], in_=xs_d[b * 128:(b + 1) * 128, :]),
                        writes=[B_xsb[s]], lane=B_xsb[s])

                def stage_T(b):
                    s = b % 2
                    sx = b % NX
                    for c in range(8):
                        op("tensor", lambda e: e.transpose(out=PTB[:, c, :], in_=xsb[sx][:, c * 128:(c + 1) * 128],
                                                           identity=identb[:]),
                           reads=[B_xsb[sx], B_const], writes=[B_PTB])
                    op("vector", lambda e: e.tensor_copy(out=XBT[s][:, 0:4, :], in_=PTB[:, 0:4, :]),
                       reads=[B_PTB], writes=[B_XBT[s]])
                    op("scalar", lambda e: e.activation(out=XBT[s][:, 4:8, :], in_=PTB[:, 4:8, :], func=AF.Copy),
                       reads=[B_PTB], writes=[B_XBT[s]])

                def stage_GU(b):
                    s, w = b % 2, (b // 2) % NW
                    pg_, pu_ = 0 + 2 * s, 1 + 2 * s
                    for (pi, wt) in ((pg_, WG[w]), (pu_, WU[w])):
                        for c in range(8):
                            mm(P[pi][:], XBT[s][:, c, :], wt[:, c, :], c == 0, c == 7,
                               [B_Wgu[w], B_XBT[s]], [B_P[pi]])
                    op("scalar", lambda e: e.activation(out=SG[s][:], in_=P[pg_][:], func=AF.Silu),
                       reads=[B_P[pg_]], writes=[B_SG[s]])
                    op("vector", lambda e: e.tensor_tensor(out=HID[s][:], in0=P[pu_][:], in1=SG[s][:], op=ALU.mult),
                       reads=[B_P[pu_], B_SG[s]], writes=[B_HID[s]])

                def stage_HT(b):
                    s = b % 2
                    for f in range(4):
                        op("tensor", lambda e: e.transpose(out=PTH[:, f, :], in_=HID[s][:, f * 128:(f + 1) * 128],
                                                           identity=identb[:]),
                           reads=[B_HID[s], B_const], writes=[B_PTH])
                    op("vector", lambda e: e.tensor_copy(out=HT[s][:], in_=PTH[:]),
                       reads=[B_PTH], writes=[B_HT[s]])

                def stage_D(b):
                    s, w = b % 2, (b // 2) % NW
                    for h2 in range(2):
                        for f in range(4):
                            mm(P[4 + h2][:], HT[s][:, f, :], WD[w][:, f, h2 * 512:(h2 + 1) * 512], f == 0, f == 3,
                               [B_HT[s], B_Wd[w]], [B_P[4 + h2]])
                    op("scalar", lambda e: e.activation(out=YB[s][:, 0:512], in_=P[4][:], func=AF.Copy),
                       reads=[B_P[4]], writes=[B_YB[s]])
                    op("vector", lambda e: e.tensor_copy(out=YB[s][:, 512:1024], in_=P[5][:]),
                       reads=[B_P[5]], writes=[B_YB[s]])
                    dma("sync", lambda e: e.dma_start(out=ys_d[b * 128:(b + 1) * 128, :], in_=YB[s][:]),
                        reads=[B_YB[s]], lane=B_YB[s])

                NB_ = NBLK_RUN
                NTL = 2 * NB_
                for w0 in range(min(3, NB_)):
                    wload(w0)
                for x0 in range(min(5, NTL)):
                    xload(x0)
                stage_T(0)
                stage_GU(0)
                if NTL > 1:
                    stage_T(1)
                for rt_ in range(NTL):
                    if rt_ % 2 == 0 and rt_ // 2 + 3 < NB_:
                        wload(rt_ // 2 + 3)
                    if rt_ + 5 < NTL:
                        xload(rt_ + 5)
                    if rt_ + 1 < NTL:
                        stage_GU(rt_ + 1)
                    stage_HT(rt_)
                    if rt_ + 2 < NTL:
                        stage_T(rt_ + 2)
                    stage_D(rt_)
                T.barrier()
                if STOP == "D1":
                    return

            with contextlib.ExitStack() as cD2:
                lng2 = sbt(cD2, "lng2", [128, D], F32)
                lnb2 = sbt(cD2, "lnb2", [128, D], F32)
                B_w2 = T.buf("w2")
                dma("sync", lambda e: e.dma_start(out=lng2[:], in_=ln2g_d.broadcast_to([128, D])), writes=[B_w2], lane=B_w2)
                dma("sync", lambda e: e.dma_start(out=lnb2[:], in_=ln2b_d.broadcast_to([128, D])), writes=[B_w2], lane=B_w2)
                Y1 = [sbt(cD2, "Y1_%d" % i, [128, D], F32) for i in range(3)]
                Y2 = [sbt(cD2, "Y2_%d" % i, [128, D], F32) for i in range(3)]
                B_Y1 = [T.buf("Y1%d" % i) for i in range(3)]
                B_Y2 = [T.buf("Y2%d" % i) for i in range(3)]
                R2s = [sbt(cD2, "R2_%d" % i, [128, D], F32) for i in range(2)]
                B_R2s = [T.buf("R2_0"), T.buf("R2_1")]
                OU = [sbt(cD2, "OU%d" % i, [128, D], F32) for i in range(2)]
                B_OU = [T.buf("OU0"), T.buf("OU1")]
                sts = [sbt(cD2, "st2_%d" % i, [128, 2, 6], F32) for i in range(2)]
                mvs = [sbt(cD2, "mv2_%d" % i, [128, 2], F32) for i in range(2)]
                rstds = [sbt(cD2, "rstd2_%d" % i, [128, 1], F32) for i in range(2)]
                nbs = [sbt(cD2, "nbias2_%d" % i, [128, 1], F32) for i in range(2)]
                vepss = [sbt(cD2, "veps2_%d" % i, [128, 1], F32) for i in range(2)]
                B_sts = [T.buf("st2_0"), T.buf("st2_1")]
                B_mvs = [T.buf("mv2_0"), T.buf("mv2_1")]
                B_rstds = [T.buf("rstd2_0"), T.buf("rstd2_1")]
                B_nbs = [T.buf("nb2_0"), T.buf("nb2_1")]
                B_vepss = [T.buf("veps2_0"), T.buf("veps2_1")]
                mhalf2 = sbt(cD2, "mhalf2", [128, 1], F32)
                B_mh2 = T.buf("mhalf2")
                op("gpsimd", lambda e: e.memset(mhalf2[:], -0.5), writes=[B_mh2])

                def loadD2(i):
                    s = i % 3
                    dma("sync", lambda e: e.dma_start(out=hin[s][:], in_=h1_d[i * 128:(i + 1) * 128, :]),
                        writes=[B_hin[s]], lane=B_hin[s])
                    for (Yt, By, Di) in ((Y1, B_Y1, D1i), (Y2, B_Y2, D2i)):
                        dma("gpsimd", lambda e: e.indirect_dma_start(
                            out=Yt[s][:], out_offset=None, in_=ys_d[:, :],
                            in_offset=bass.IndirectOffsetOnAxis(ap=Di[:, i:i + 1], axis=0)),
                            reads=[B_rt], writes=[By[s]], lane=By[s])

                def d2_part0(i):
                    s, s3 = i % 2, i % 3
                    op("scalar", lambda e: e.activation(out=R2s[s][:], in_=hin[s3][:], func=AF.Copy, scale=ALPHA),
                       reads=[B_hin[s3]], writes=[B_R2s[s]])

                def d2_part1(i):
                    s, s3 = i % 2, i % 3
                    R2, B_R2, st, mv, rstd, nbias2, veps2 = R2s[s], B_R2s[s], sts[s], mvs[s], rstds[s], nbs[s], vepss[s]
                    B_st, B_mv, B_rstd, B_nb2, B_veps2 = B_sts[s], B_mvs[s], B_rstds[s], B_nbs[s], B_vepss[s]
                    op("vector", lambda e: e.scalar_tensor_tensor(out=R2[:], in0=Y1[s3][:], scalar=G1[:, i:i + 1],
                                                                  in1=R2[:], op0=ALU.mult, op1=ALU.add),
                       reads=[B_Y1[s3], B_R2, B_rt], writes=[B_R2])
                    op("vector", lambda e: e.scalar_tensor_tensor(out=R2[:], in0=Y2[s3][:], scalar=G2[:, i:i + 1],
                                                                  in1=R2[:], op0=ALU.mult, op1=ALU.add),
                       reads=[B_Y2[s3], B_R2, B_rt], writes=[B_R2])
                    for c in range(2):
                        op("vector", lambda e: e.bn_stats(out=st[:, c, :], in_=R2[:, c * 512:(c + 1) * 512]),
                           reads=[B_R2], writes=[B_st])
                    op("vector", lambda e: e.bn_aggr(out=mv[:, :], in_=st[:, :, :].rearrange("p a b -> p (a b)")),
                       reads=[B_st], writes=[B_mv])
                    op("gpsimd", lambda e: e.tensor_scalar(out=veps2[:], in0=mv[:, 1:2], scalar1=EPS, scalar2=None,
                                                           op0=ALU.add), reads=[B_mv], writes=[B_veps2])
                    op("gpsimd", lambda e: e.tensor_tensor(out=rstd[:], in0=veps2[:], in1=mhalf2[:], op=ALU.pow),
                       reads=[B_veps2, B_mh2], writes=[B_rstd])

                def d2_part1b(i):
                    s = i % 2
                    R2, B_R2, mv, rstd, nbias2 = R2s[s], B_R2s[s], mvs[s], rstds[s], nbs[s]
                    B_mv, B_rstd, B_nb2 = B_mvs[s], B_rstds[s], B_nbs[s]
                    op("vector", lambda e: e.scalar_tensor_tensor(out=nbias2[:], in0=mv[:, 0:1], scalar=-1.0, in1=rstd[:],
                                                                  op0=ALU.mult, op1=ALU.mult),
                       reads=[B_mv, B_rstd], writes=[B_nb2])
                    op("scalar", lambda e: e.activation(out=OU[s][:], in_=R2[:], func=AF.Identity, scale=rstd[:, 0:1],
                                                        bias=nbias2[:, 0:1]),
                       reads=[B_R2, B_nb2, B_rstd], writes=[B_OU[s]])

                def d2_part2(i):
                    s = i % 2
                    op("vector", lambda e: e.tensor_tensor(out=OU[s][:], in0=OU[s][:], in1=lng2[:], op=ALU.mult),
                       reads=[B_OU[s], B_w2], writes=[B_OU[s]])
                    op("vector", lambda e: e.tensor_tensor(out=OU[s][:], in0=OU[s][:], in1=lnb2[:], op=ALU.add),
                       reads=[B_OU[s], B_w2], writes=[B_OU[s]])
                    dma("sync", lambda e: e.dma_start(out=out_d[i * 128:(i + 1) * 128, :], in_=OU[s][:]),
                        reads=[B_OU[s]], lane=B_OU[s])

                loadD2(0)
                loadD2(1)
                loadD2(2)
                d2_part0(0)
                d2_part1(0)
                d2_part1b(0)
                d2_part0(1)
                for i in range(NT):
                    if i + 3 < NT:
                        loadD2(i + 3)
                    if i + 2 < NT:
                        d2_part0(i + 2)
                    if i + 1 < NT:
                        d2_part1(i + 1)
                    d2_part2(i)
                    if i + 1 < NT:
                        d2_part1b(i + 1)
                T.barrier()

    with contextlib.ExitStack() as es:
        _body(es)
    return nc


def _kc(w, nk):
    n = w.shape[1]
    return np.ascontiguousarray(w.reshape(nk, 128, n).transpose(1, 0, 2))


def _const_tables(hf):
    inv = 1.0 / (10000.0 ** (np.arange(0, 32, 2, dtype=np.float64) / 32.0))
    pos_all = np.arange(S, dtype=np.float64)
    ang = pos_all[:, None] * inv[None, :]
    cos_a, sin_a = np.cos(ang), np.sin(ang)
    sign = np.concatenate([-np.ones(16), np.ones(16)])
    cos32 = np.concatenate([cos_a, cos_a], axis=1).T
    sin32 = (np.concatenate([sin_a, sin_a], axis=1) * sign[None, :]).T
    cosk = np.concatenate([cos32, cos32], axis=0).astype(np.float32)
    sink = np.concatenate([sin32, sin32], axis=0).astype(np.float32)
    loc = np.arange(NOWN)
    own_pos = (loc // 32) * 64 + hf * 32 + (loc % 32)
    tblq = np.empty((128, NOWN), np.float64)
    tblq[0:32] = cos32[:, own_pos] * SCALE
    tblq[32:64] = sin32[:, own_pos] * SCALE
    tblq[64:128] = SCALE
    wins = (2, 4, 8, 16)
    a_main = np.zeros((128, 4, 64)); a_halo = np.zeros((128, 4, 64)); a_first = np.zeros((128, 4, 64))
    for g, w in enumerate(wins):
        for j in range(64):
            t = 64 * (j // 32) + 32 * hf + (j % 32)
            for tp in range(t - w + 1, t + 1):
                if tp >= 0:
                    a_main[tp, g, j] += 1.0 / w
                    a_first[tp, g, j] += 1.0 / min(t + 1, w)
                else:
                    a_halo[128 + tp, g, j] += 1.0 / w
            a_main[t, g, j] -= 1.0
            a_first[t, g, j] -= 1.0
    return (cosk, sink, tblq.astype(np.float32), a_main.astype(np.float32), a_halo.astype(np.float32),
            a_first.astype(np.float32), own_pos)


def _prep_shared(w_in, pool_mix_w, pool_scale, q_norm_g, w_uq, kv_norm_g, w_ukv, w_mla_o, w_out,
                 ln1_g, ln1_b, w_router_group, b_router_group, w_router_expert, b_router_expert,
                 w_gate, w_up, w_down, ln2_g, ln2_b):
    f = lambda a: np.asarray(a, dtype=np.float32)
    w_in = f(w_in)[0]
    sh = {}
    sh["w_pool"] = _kc(w_in[:, 0:512], 8)
    sh["w_cq"] = _kc(w_in[:, 512:896], 8)
    sh["w_ckv"] = _kc(w_in[:, 896:1152], 8)
    kpe = w_in[:, 1152:1184]
    kpesw = np.concatenate([kpe[:, 16:32], kpe[:, 0:16]], axis=1)
    sh["w_kpe2"] = _kc(np.concatenate([kpe, kpe], axis=1), 8)
    sh["w_kpesw2"] = _kc(np.concatenate([kpesw, kpesw], axis=1), 8)
    sh["w_gates"] = _kc(w_in[:, 1184:3232], 8)
    uq = f(w_uq)[0].reshape(384, 8, 96)
    nope, pe = uq[:, :, 0:64], uq[:, :, 64:96]
    pesw = np.concatenate([pe[:, :, 16:32], pe[:, :, 0:16]], axis=2)
    uq_l = np.concatenate([pe, pesw, nope], axis=2)
    sh["w_uq_l"] = np.ascontiguousarray(uq_l.reshape(3, 128, 8, 128).transpose(1, 0, 2, 3))
    sh["qg"] = np.ascontiguousarray(f(q_norm_g)[0].reshape(3, 128).T)
    ukv = f(w_ukv)[0].reshape(256, 8, 128)
    sh["w_uk_l"] = np.ascontiguousarray(ukv[:, :, 0:64].reshape(2, 128, 8, 64).transpose(1, 0, 2, 3))
    sh["w_uv_l"] = np.ascontiguousarray(ukv[:, :, 64:128].reshape(2, 128, 8, 64).transpose(1, 0, 2, 3))
    sh["kvg"] = np.ascontiguousarray(f(kv_norm_g)[0].reshape(2, 128).T)
    sh["w_mo_l"] = _kc(f(w_mla_o)[0], 4)
    sh["w_out_l"] = _kc(f(w_out)[0], 8)
    sh["w_pm_l"] = np.ascontiguousarray(f(pool_mix_w)[0].transpose(1, 0, 2))
    sh["psc"] = np.ascontiguousarray(f(pool_scale)[0].reshape(8, 128).T)
    sh["ln1_g"] = f(ln1_g).reshape(1, D)
    sh["ln1_b"] = f(ln1_b).reshape(1, D)
    sh["ln2_g"] = f(ln2_g).reshape(1, D)
    sh["ln2_b"] = f(ln2_b).reshape(1, D)
    sh["w_r"] = _kc(np.concatenate([f(w_router_group)[0], f(w_router_expert)[0]], axis=1), 8)
    sh["b_r"] = np.concatenate([f(b_router_group)[0], f(b_router_expert)[0]]).reshape(1, 36)
    wg = f(w_gate)[0]
    wu = f(w_up)[0]
    wd = f(w_down)[0]
    sh["wg_l"] = np.ascontiguousarray(wg.reshape(32, 8, 128, 512).transpose(0, 2, 1, 3)).reshape(32 * 128, 4096)
    sh["wu_l"] = np.ascontiguousarray(wu.reshape(32, 8, 128, 512).transpose(0, 2, 1, 3)).reshape(32 * 128, 4096)
    sh["wd_l"] = np.ascontiguousarray(wd.reshape(32, 4, 128, 1024).transpose(0, 2, 1, 3)).reshape(32 * 128, 4096)
    sh["ident"] = np.eye(128, dtype=np.float32)
    sh["lstrict"] = np.triu(np.ones((128, 128), np.float32), k=1)
    return sh


def make_in_maps(inputs):
    x = np.asarray(inputs["x"], dtype=np.float32)
    sh = _prep_shared(**{k: v for k, v in inputs.items() if k != "x"})
    in_maps, own_positions = [], []
    consts = [_const_tables(hf) for hf in range(2)]
    for core in range(8):
        b, hf = core // 2, core % 2
        cosk, sink, tblq, a_main, a_halo, a_first, own_pos = consts[hf]
        xb = x[b]
        m = dict(sh)
        m["xT_all"] = np.ascontiguousarray(xb.T.reshape(8, 128, S).transpose(1, 0, 2))
        xo = xb[own_pos]
        m["x_own"] = np.ascontiguousarray(xo)
        m["xT_own"] = np.ascontiguousarray(xo.T.reshape(8, 128, NOWN).transpose(1, 0, 2))
        m["cosk"], m["sink"], m["tblq"] = cosk, sink, tblq
        m["a_main"], m["a_halo"], m["a_first"] = a_main, a_halo, a_first
        in_maps.append(m)
        own_positions.append(own_pos)
    return in_maps, own_positions


_NC_CACHE = {}


def kernel(**inputs):
    in_maps, own_positions = make_in_maps(inputs)
    if "nc" not in _NC_CACHE:
        _NC_CACHE["nc"] = build_program()
    nc = _NC_CACHE["nc"]
    res = run_bass_kernel_spmd(nc, in_maps, core_ids=list(range(8)))
    out = np.empty((B, S, D), np.float32)
    for core in range(8):
        b = core // 2
        out[b, own_positions[core], :] = np.asarray(res.results[core]["out"], dtype=np.float32)
    if DEBUG:
        kernel.last_results = res.results
    return out
```
